# Optimizing a Trainium2 kernel written in Bass

```python
import jax
import jax.numpy as jnp
from jax import lax
import numpy as np

D_MODEL = 1024
BATCH = 8
SEQ = 4096
DEPTH = 2

GRID_W = 64
CTX_LEN = 256
HEAD_DIM = 64
ROPE_THETA = 10000.0
EPS = 1e-6
Q_BLOCK = 128
NEG_BIG = -1e30

A_HEADS = 6
A_KV_HEADS = 2
B_HEADS = 4
B_Q_RANK = 256
B_KV_RANK = 128
B_NOPE = 64
B_ROPE = 32
B_V = 64
C_HEADS = 6
C_KV_HEADS = 2
C_WINDOW = 128

A_COLS = (A_HEADS + 2 * A_KV_HEADS) * HEAD_DIM
B_COLS = B_Q_RANK + B_KV_RANK + B_ROPE
C_COLS = (C_HEADS + 2 * C_KV_HEADS) * HEAD_DIM
IN_COLS = A_COLS + B_COLS + C_COLS
MIX_WIDTH = A_HEADS * HEAD_DIM + B_HEADS * B_V + C_HEADS * HEAD_DIM

D_FF_DENSE = 2816
N_EXPERTS = 8
TOP_K = 2
D_FF_EXPERT = 3584
N_DENSE = (DEPTH + 1) // 2
N_MOE = DEPTH // 2

kernel_name = "hybrid_parallel_heads_diffusion_block"


def rmsnorm(x, g):
    xf = x.astype(jnp.float32)
    y = xf * lax.rsqrt(jnp.mean(xf * xf, axis=-1, keepdims=True) + EPS)
    return (y * g.astype(jnp.float32)).astype(x.dtype)


def axial_rope_tables(n_tok, rot_dim, dtype):
    rows = n_tok // GRID_W
    row, col = jnp.meshgrid(jnp.arange(rows), jnp.arange(GRID_W), indexing="ij")
    row = row.reshape(-1).astype(jnp.float32)
    col = col.reshape(-1).astype(jnp.float32)
    n_freq = rot_dim // 4
    inv_freq = ROPE_THETA ** (-jnp.arange(n_freq, dtype=jnp.float32) / n_freq)
    ang_r = row[:, None] * inv_freq
    ang_c = col[:, None] * inv_freq
    ang = jnp.concatenate([ang_r, ang_r, ang_c, ang_c], axis=-1)
    return jnp.cos(ang).astype(dtype), jnp.sin(ang).astype(dtype)


def apply_axial_rope(t, cos, sin):
    n_freq = t.shape[-1] // 4
    t4 = t.reshape(t.shape[:-1] + (2, 2, n_freq))
    rot = jnp.concatenate([-t4[..., 1:, :], t4[..., :1, :]], axis=-2).reshape(t.shape)
    return t * cos[:, None, :] + rot * sin[:, None, :]


def rope_latent(t, n_ctx, cos, sin):
    return jnp.concatenate([t[:, :n_ctx], apply_axial_rope(t[:, n_ctx:], cos, sin)], axis=1)


def to_groups(q, n_kv):
    b, t, h, d = q.shape
    return q.reshape(b, t, n_kv, h // n_kv, d).transpose(0, 2, 3, 1, 4)


def to_heads(k):
    return k.transpose(0, 2, 1, 3)


def merge_heads(o):
    b, hk, g, t, d = o.shape
    return o.transpose(0, 3, 1, 2, 4).reshape(b, t, hk * g * d)


def sdpa(q, k, v, scale):
    s = jnp.einsum("bhgqd,bhkd->bhgqk", q, k, preferred_element_type=jnp.float32) * scale
    p = jax.nn.softmax(s, axis=-1)
    return jnp.einsum("bhgqk,bhkd->bhgqd", p.astype(v.dtype), v)


def block_sweep_attention(q, k, v, scale):
    b, hk, g, s, dk = q.shape
    nb = s // Q_BLOCK
    qb = jnp.moveaxis(q.reshape(b, hk, g, nb, Q_BLOCK, dk), 3, 0)
    ob = lax.map(lambda qi: sdpa(qi, k, v, scale), qb)
    return jnp.moveaxis(ob, 0, 3).reshape(b, hk, g, s, v.shape[-1])


def sink_softmax(scores, sink):
    m = sink
    for s in scores:
        m = jnp.maximum(m, jnp.max(s, axis=-1, keepdims=True))
    e = [jnp.exp(s - m) for s in scores]
    denom = jnp.exp(sink - m)
    for ei in e:
        denom = denom + jnp.sum(ei, axis=-1, keepdims=True)
    return [ei / denom for ei in e]


def global_gqa(qkv, n_ctx, q_gain, k_gain, cos, sin, with_ctx):
    b, t, _ = qkv.shape
    q, k, v = jnp.split(qkv, [A_HEADS * HEAD_DIM, (A_HEADS + A_KV_HEADS) * HEAD_DIM], axis=-1)
    q = rope_latent(rmsnorm(q.reshape(b, t, A_HEADS, HEAD_DIM), q_gain), n_ctx, cos, sin)
    k = rope_latent(rmsnorm(k.reshape(b, t, A_KV_HEADS, HEAD_DIM), k_gain), n_ctx, cos, sin)
    v = v.reshape(b, t, A_KV_HEADS, HEAD_DIM)
    qg, kh, vh = to_groups(q, A_KV_HEADS), to_heads(k), to_heads(v)
    scale = HEAD_DIM ** -0.5
    out_lat = merge_heads(block_sweep_attention(qg[:, :, :, n_ctx:], kh, vh, scale))
    out_ctx = None
    if with_ctx:
        out_ctx = merge_heads(sdpa(qg[:, :, :, :n_ctx], kh[:, :, :n_ctx], vh[:, :, :n_ctx], scale))
    return out_ctx, out_lat


def latent_attention(proj, n_ctx, q_gain, kv_gain, w_uq, w_ukv, cos, sin, with_ctx):
    b, t, _ = proj.shape
    cq, ckv, k_rope = jnp.split(proj, [B_Q_RANK, B_Q_RANK + B_KV_RANK], axis=-1)
    q = (rmsnorm(cq, q_gain) @ w_uq).reshape(b, t, B_HEADS, B_NOPE + B_ROPE)
    kv = (rmsnorm(ckv, kv_gain) @ w_ukv).reshape(b, t, B_HEADS, B_NOPE + B_V)
    q_nope, q_rope = jnp.split(q, [B_NOPE], axis=-1)
    k_nope, v = jnp.split(kv, [B_NOPE], axis=-1)
    q_rope = rope_latent(q_rope, n_ctx, cos, sin)
    k_rope = rope_latent(k_rope[:, :, None, :], n_ctx, cos, sin)
    q = jnp.concatenate([q_nope, q_rope], axis=-1)
    k = jnp.concatenate([k_nope, jnp.broadcast_to(k_rope, (b, t, B_HEADS, B_ROPE))], axis=-1)
    qg, kh, vh = to_groups(q, B_HEADS), to_heads(k), to_heads(v)
    scale = (B_NOPE + B_ROPE) ** -0.5
    out_lat = merge_heads(block_sweep_attention(qg[:, :, :, n_ctx:], kh, vh, scale))
    out_ctx = None
    if with_ctx:
        out_ctx = merge_heads(sdpa(qg[:, :, :, :n_ctx], kh[:, :, :n_ctx], vh[:, :, :n_ctx], scale))
    return out_ctx, out_lat


def window_sink_gqa(qkv, n_ctx, sinks, cos, sin, with_ctx):
    b, t, _ = qkv.shape
    q, k, v = jnp.split(qkv, [C_HEADS * HEAD_DIM, (C_HEADS + C_KV_HEADS) * HEAD_DIM], axis=-1)
    q = rope_latent(q.reshape(b, t, C_HEADS, HEAD_DIM), n_ctx, cos, sin)
    k = rope_latent(k.reshape(b, t, C_KV_HEADS, HEAD_DIM), n_ctx, cos, sin)
    v = v.reshape(b, t, C_KV_HEADS, HEAD_DIM)
    qg, kh, vh = to_groups(q, C_KV_HEADS), to_heads(k), to_heads(v)
    g = C_HEADS // C_KV_HEADS
    scale = HEAD_DIM ** -0.5
    sink = sinks.astype(jnp.float32).reshape(C_KV_HEADS, g)
    k_ctx, v_ctx = kh[:, :, :n_ctx], vh[:, :, :n_ctx]
    q_lat, k_lat, v_lat = qg[:, :, :, n_ctx:], kh[:, :, n_ctx:], vh[:, :, n_ctx:]
    s_len = t - n_ctx
    w = C_WINDOW
    nb = s_len // w
    qb = q_lat.reshape(b, C_KV_HEADS, g, nb, w, HEAD_DIM)

    def band(u):
        up = jnp.pad(u, ((0, 0), (0, 0), (w, w), (0, 0))).reshape(b, C_KV_HEADS, nb + 2, w, u.shape[-1])
        return jnp.concatenate([up[:, :, :-2], up[:, :, 1:-1], up[:, :, 2:]], axis=-2)

    kb, vb = band(k_lat), band(v_lat)
    blk = jnp.arange(nb)[:, None] * w
    q_pos = blk + jnp.arange(w)[None, :]
    k_pos = blk - w + jnp.arange(3 * w)[None, :]
    rel = k_pos[:, None, :] - q_pos[:, :, None]
    valid = (jnp.abs(rel) <= C_WINDOW) & (k_pos[:, None, :] >= 0) & (k_pos[:, None, :] < s_len)
    s_loc = jnp.einsum("bhgnqd,bhnkd->bhgnqk", qb, kb, preferred_element_type=jnp.float32) * scale
    s_loc = jnp.where(valid, s_loc, NEG_BIG)
    s_ctx = jnp.einsum("bhgnqd,bhkd->bhgnqk", qb, k_ctx, preferred_element_type=jnp.float32) * scale
    p_loc, p_ctx = sink_softmax([s_loc, s_ctx], sink[None, :, :, None, None, None])
    o = (jnp.einsum("bhgnqk,bhnkd->bhgnqd", p_loc.astype(vb.dtype), vb)
         + jnp.einsum("bhgnqk,bhkd->bhgnqd", p_ctx.astype(v_ctx.dtype), v_ctx))
    out_lat = merge_heads(o.reshape(b, C_KV_HEADS, g, s_len, HEAD_DIM))
    out_ctx = None
    if with_ctx:
        s_cc = jnp.einsum("bhgqd,bhkd->bhgqk", qg[:, :, :, :n_ctx], k_ctx,
                          preferred_element_type=jnp.float32) * scale
        (p_cc,) = sink_softmax([s_cc], sink[None, :, :, None, None])
        out_ctx = merge_heads(jnp.einsum("bhgqk,bhkd->bhgqd", p_cc.astype(v_ctx.dtype), v_ctx))
    return out_ctx, out_lat


def swiglu(t, w1, w3, w2):
    return (jax.nn.silu(t @ w1) * (t @ w3)) @ w2


def moe_swiglu(t, w_router, w1, w3, w2):
    shape = t.shape
    tok = t.reshape(-1, shape[-1])
    logits = jnp.dot(tok, w_router, preferred_element_type=jnp.float32)
    top_logit, top_idx = lax.top_k(logits, TOP_K)
    gates = jax.nn.softmax(top_logit, axis=-1)
    combine = jnp.sum(jax.nn.one_hot(top_idx, N_EXPERTS, dtype=jnp.float32) * gates[..., None],
                      axis=1).astype(tok.dtype)
    out = jnp.zeros_like(tok)
    for e in range(N_EXPERTS):
        out = out + combine[:, e:e + 1] * swiglu(tok, w1[e], w3[e], w2[e])
    return out.reshape(shape)


def channel_mixer(t, layer, w1_dense, w3_dense, w2_dense, w_router, w1_moe, w3_moe, w2_moe):
    j = layer // 2
    if layer % 2 == 0:
        return swiglu(t, w1_dense[j], w3_dense[j], w2_dense[j])
    return moe_swiglu(t, w_router[j], w1_moe[j], w3_moe[j], w2_moe[j])


def setup_inputs(seed: int = 0) -> dict:
    key = jax.random.key(seed)
    ks = jax.random.split(key, 25)
    f32 = jnp.float32

    def nrm(k, shape, scale=1.0):
        return jax.random.normal(k, shape, f32) * scale

    def gain(k, shape):
        return 1.0 + 0.1 * jax.random.normal(k, shape, f32)

    return {
        "x": nrm(ks[0], (BATCH, SEQ, D_MODEL)),
        "c": nrm(ks[1], (BATCH, D_MODEL)),
        "ctx": nrm(ks[2], (BATCH, CTX_LEN, D_MODEL)),
        "c_ctx": nrm(ks[3], (D_MODEL,)),
        "w_mod": nrm(ks[4], (DEPTH, D_MODEL, 6 * D_MODEL), 0.5 * D_MODEL ** -0.5),
        "b_mod": nrm(ks[5], (DEPTH, 6 * D_MODEL), 0.01),
        "norm_mix": gain(ks[6], (DEPTH, D_MODEL)),
        "norm_ffn": gain(ks[7], (DEPTH, D_MODEL)),
        "w_in": nrm(ks[8], (DEPTH, D_MODEL, IN_COLS), D_MODEL ** -0.5),
        "a_q_norm": gain(ks[9], (DEPTH, HEAD_DIM)),
        "a_k_norm": gain(ks[10], (DEPTH, HEAD_DIM)),
        "b_q_norm": gain(ks[11], (DEPTH, B_Q_RANK)),
        "b_kv_norm": gain(ks[12], (DEPTH, B_KV_RANK)),
        "w_uq": nrm(ks[13], (DEPTH, B_Q_RANK, B_HEADS * (B_NOPE + B_ROPE)), B_Q_RANK ** -0.5),
        "w_ukv": nrm(ks[14], (DEPTH, B_KV_RANK, B_HEADS * (B_NOPE + B_V)), B_KV_RANK ** -0.5),
        "c_sinks": nrm(ks[15], (DEPTH, C_HEADS)),
        "w_out": nrm(ks[16], (DEPTH, MIX_WIDTH, D_MODEL), MIX_WIDTH ** -0.5),
        "w1_dense": nrm(ks[17], (N_DENSE, D_MODEL, D_FF_DENSE), D_MODEL ** -0.5),
        "w3_dense": nrm(ks[18], (N_DENSE, D_MODEL, D_FF_DENSE), D_MODEL ** -0.5),
        "w2_dense": nrm(ks[19], (N_DENSE, D_FF_DENSE, D_MODEL), D_FF_DENSE ** -0.5),
        "w_router": nrm(ks[20], (N_MOE, D_MODEL, N_EXPERTS), D_MODEL ** -0.5),
        "w1_moe": nrm(ks[21], (N_MOE, N_EXPERTS, D_MODEL, D_FF_EXPERT), D_MODEL ** -0.5),
        "w3_moe": nrm(ks[22], (N_MOE, N_EXPERTS, D_MODEL, D_FF_EXPERT), D_MODEL ** -0.5),
        "w2_moe": nrm(ks[23], (N_MOE, N_EXPERTS, D_FF_EXPERT, D_MODEL), D_FF_EXPERT ** -0.5),
        "final_norm": gain(ks[24], (D_MODEL,)),
    }


def reference(x, c, ctx, c_ctx, w_mod, b_mod, norm_mix, norm_ffn, w_in, a_q_norm, a_k_norm,
              b_q_norm, b_kv_norm, w_uq, w_ukv, c_sinks, w_out, w1_dense, w3_dense, w2_dense,
              w_router, w1_moe, w3_moe, w2_moe, final_norm):
    n_ctx = ctx.shape[1]
    n_lat = x.shape[1]
    cos_h, sin_h = axial_rope_tables(n_lat, HEAD_DIM, x.dtype)
    cos_r, sin_r = axial_rope_tables(n_lat, B_ROPE, x.dtype)
    xc = ctx
    for i in range(DEPTH):
        last = i == DEPTH - 1
        mod_lat = jnp.split(jax.nn.silu(c) @ w_mod[i] + b_mod[i], 6, axis=-1)
        sh_a, sc_a, gt_a, sh_f, sc_f, gt_f = [m[:, None, :] for m in mod_lat]
        csh_a, csc_a, cgt_a, csh_f, csc_f, cgt_f = jnp.split(jax.nn.silu(c_ctx) @ w_mod[i] + b_mod[i], 6, axis=-1)

        h = jnp.concatenate([rmsnorm(xc, norm_mix[i]) * (1 + csc_a) + csh_a,
                             rmsnorm(x, norm_mix[i]) * (1 + sc_a) + sh_a], axis=1)
        proj = h @ w_in[i]
        p_a, p_b, p_c = jnp.split(proj, [A_COLS, A_COLS + B_COLS], axis=-1)
        a_ctx, a_lat = global_gqa(p_a, n_ctx, a_q_norm[i], a_k_norm[i], cos_h, sin_h, not last)
        b_ctx, b_lat = latent_attention(p_b, n_ctx, b_q_norm[i], b_kv_norm[i], w_uq[i], w_ukv[i],
                                        cos_r, sin_r, not last)
        c_ctx_out, c_lat = window_sink_gqa(p_c, n_ctx, c_sinks[i], cos_h, sin_h, not last)
        x = x + gt_a * (jnp.concatenate([a_lat, b_lat, c_lat], axis=-1) @ w_out[i])
        if not last:
            xc = xc + cgt_a * (jnp.concatenate([a_ctx, b_ctx, c_ctx_out], axis=-1) @ w_out[i])

        hf_lat = rmsnorm(x, norm_ffn[i]) * (1 + sc_f) + sh_f
        if last:
            x = x + gt_f * channel_mixer(hf_lat, i, w1_dense, w3_dense, w2_dense,
                                         w_router, w1_moe, w3_moe, w2_moe)
        else:
            hf = jnp.concatenate([rmsnorm(xc, norm_ffn[i]) * (1 + csc_f) + csh_f, hf_lat], axis=1)
            y = channel_mixer(hf, i, w1_dense, w3_dense, w2_dense, w_router, w1_moe, w3_moe, w2_moe)
            xc = xc + cgt_f * y[:, :n_ctx]
            x = x + gt_f * y[:, n_ctx:]
    return rmsnorm(x, final_norm)
```

```python
import contextlib
import numpy as np
import ml_dtypes
import concourse.bass as bass
import concourse.mybir as mybir
from concourse.bass_utils import run_bass_kernel_spmd

F32 = mybir.dt.float32
BF16 = mybir.dt.bfloat16
AF = mybir.ActivationFunctionType
ALU = mybir.AluOpType
AX = mybir.AxisListType

D = 1024
NCTX = 256
SEQ = 4096
T = NCTX + SEQ
NT = T // 128
DEPTH = 2
HD = 64
EPS = 1e-6
DFF_D = 2816
DFF_E = 3584
NE = 8
BLOCKS = [(0, 256)] + [(256 + 512 * i, 512) for i in range(8)]


class Buf:
    __slots__ = ("name", "w", "r", "dsem")

    def __init__(self, name, dsem=None):
        self.name = name
        self.w = None
        self.r = {}
        self.dsem = dsem


class Sync:
    ENG = ("pe", "act", "dve", "pool", "sp")

    def __init__(self, nc, es, n_dma_sems=56):
        self.nc = nc
        self.eng = {"pe": nc.tensor, "act": nc.scalar, "dve": nc.vector, "pool": nc.gpsimd, "sp": nc.sync}
        self.semobj = {}
        self.cnt = {}
        for e in ("pe", "act", "dve", "pool"):
            self.semobj[e] = es.enter_context(nc.semaphore("s_" + e))
            self.cnt[e] = 0
        self.dma_keys = []
        for i in range(n_dma_sems):
            k = "d%d" % i
            self.semobj[k] = es.enter_context(nc.semaphore("s_" + k))
            self.cnt[k] = 0
            self.dma_keys.append(k)
        self.next_dma = 0
        self.next_sw = 0
        self.n_sw = 8
        self.known = {e: {} for e in self.ENG}
        self.pend_r = []
        self.pend_w = []

    def new_dsem(self, sw=False):
        if sw:
            k = self.dma_keys[self.next_sw % self.n_sw]
            self.next_sw += 1
            return k
        n = len(self.dma_keys) - self.n_sw
        k = self.dma_keys[self.n_sw + self.next_dma % n]
        self.next_dma += 1
        return k

    def buf(self, name, dma=False, sw=False):
        return Buf(name, self.new_dsem(sw) if (dma or sw) else None)

    def _wait(self, e, key, val):
        if self.known[e].get(key, 0) >= val:
            return
        self.eng[e].wait_ge(self.semobj[key], val)
        self.known[e][key] = val

    def _deps(self, e, reads, writes):
        need = {}
        for b in reads:
            if b.w is not None:
                k, v = b.w
                need[k] = max(need.get(k, 0), v)
        for b in writes:
            if b.w is not None:
                k, v = b.w
                if k != e:
                    need[k] = max(need.get(k, 0), v)
            for k, v in b.r.items():
                if k != e:
                    need[k] = max(need.get(k, 0), v)
        for k, v in need.items():
            if e == "pe" and k == "pe":
                continue
            self._wait(e, k, v)

    def op(self, e, fn, reads=(), writes=(), mark=True):
        for b in list(reads) + list(writes):
            if e != "pe":
                assert not (b in self.pend_w), "pending PE write on %s" % b.name
        for b in writes:
            if e != "pe":
                assert not (b in self.pend_r), "pending PE read on %s" % b.name
        self._deps(e, reads, writes)
        ins = fn()
        if e == "pe":
            for b in reads:
                if b not in self.pend_r:
                    self.pend_r.append(b)
            for b in writes:
                if b not in self.pend_w:
                    self.pend_w.append(b)
            if mark:
                self.cnt["pe"] += 1
                ins.then_inc(self.semobj["pe"], 1)
                c = self.cnt["pe"]
                for b in self.pend_w:
                    b.w = ("pe", c)
                    b.r = {}
                for b in self.pend_r:
                    if b not in self.pend_w:
                        b.r["pe"] = c
                self.pend_r = []
                self.pend_w = []
            return ins
        self.cnt[e] += 1
        ins.then_inc(self.semobj[e], 1)
        ev = (e, self.cnt[e])
        for b in reads:
            b.r[e] = ev[1]
        for b in writes:
            b.w = ev
            b.r = {}
        return ins

    def dma(self, q, out, in_, reads=(), writes=(), key=None):
        if key is None:
            for b in list(writes) + list(reads):
                if b.dsem is not None:
                    key = b.dsem
                    break
        assert key is not None
        for b in list(reads) + list(writes):
            assert b not in self.pend_w and not (b in writes and b in self.pend_r), "pending PE on %s" % b.name
        self._deps(q, reads, writes)
        assert (q == "pool") == (self.dma_keys.index(key) < self.n_sw), "dma sem pool mismatch"
        ins = self.eng[q].dma_start(out=out, in_=in_)
        self.cnt[key] += 16
        ins.then_inc(self.semobj[key], 16)
        ev = (key, self.cnt[key])
        for b in reads:
            b.r[key] = ev[1]
        for b in writes:
            b.w = ev
            b.r = {}
        return ins

    def barrier(self):
        assert not self.pend_r and not self.pend_w
        for e in self.ENG:
            for k, v in self.cnt.items():
                if v > 0 and not (e == "pe" and k == "pe"):
                    self._wait(e, k, v)


def _rope_tables(rot_dim):
    rows = SEQ // 64
    row, col = np.meshgrid(np.arange(rows), np.arange(64), indexing="ij")
    row = row.reshape(-1).astype(np.float32)
    col = col.reshape(-1).astype(np.float32)
    n_freq = rot_dim // 4
    inv_freq = (np.float32(10000.0) ** (-np.arange(n_freq, dtype=np.float32) / np.float32(n_freq))).astype(np.float32)
    ang_r = row[:, None] * inv_freq
    ang_c = col[:, None] * inv_freq
    ang = np.concatenate([ang_r, ang_r, ang_c, ang_c], axis=-1).astype(np.float32)
    return np.cos(ang).astype(np.float32), np.sin(ang).astype(np.float32)


def _rot_perm(rot_dim):
    nf = rot_dim // 4
    P = np.zeros((rot_dim, rot_dim), np.float32)
    for a in range(2):
        b0 = a * 2 * nf
        for f in range(nf):
            P[b0 + nf + f, b0 + f] = -1.0
            P[b0 + f, b0 + nf + f] = 1.0
    return P


def make_consts():
    c = {}
    cosH, sinH = _rope_tables(64)
    cosT = np.ones((128, T), np.float32)
    sinT = np.zeros((128, T), np.float32)
    cosT[0:64, NCTX:] = cosH.T
    cosT[64:128, NCTX:] = cosH.T
    sinT[0:64, NCTX:] = sinH.T
    sinT[64:128, NCTX:] = sinH.T
    c["cosH"] = cosT
    c["sinH"] = sinT
    cosR, sinR = _rope_tables(32)
    cR = np.ones((128, T), np.float32)
    sR = np.zeros((128, T), np.float32)
    cR[64:96, NCTX:] = cosR.T
    sR[64:96, NCTX:] = sinR.T
    c["cosR"] = cR
    c["sinR"] = sR
    cK = np.ones((32, T), np.float32)
    sK = np.zeros((32, T), np.float32)
    cK[:, NCTX:] = cosR.T
    sK[:, NCTX:] = sinR.T
    c["cosK"] = cK
    c["sinK"] = sK
    P64 = _rot_perm(64)
    Pm128 = np.zeros((128, 128), np.float32)
    Pm128[0:64, 0:64] = P64
    Pm128[64:128, 64:128] = P64
    P32 = _rot_perm(32)
    Pm96 = np.zeros((96, 96), np.float32)
    Pm96[64:96, 64:96] = P32
    bo = np.zeros((128, 128), np.float32)
    bo[0:64, 0:64] = 1.0
    bo[64:128, 64:128] = 1.0
    sel = np.zeros((32, 128), np.float32)
    sel[np.arange(32), 64 + np.arange(32)] = 1.0
    cm = np.zeros((128, 7, 128), np.float32)
    cm[:, 0, :] = Pm128
    cm[0:96, 1, 0:96] = Pm96
    cm[:, 2, :] = bo
    cm[:, 3, :] = 1.0
    cm[:, 4, :] = np.eye(128, dtype=np.float32)
    cm[0:32, 5, 0:32] = P32
    cm[0:32, 6, :] = sel
    c["cmat"] = cm.astype(ml_dtypes.bfloat16)
    c["identf"] = np.eye(128, dtype=np.float32)
    i = np.arange(128)[:, None]
    j = np.arange(512)[None, :]
    m = np.zeros((128, 6, 512), np.float32)
    for ci, cc in enumerate(range(-1, 5)):
        rel = cc * 128 + i - j
        m[:, ci, :] = (np.abs(rel) <= 128).astype(np.float32)
    c["wmask"] = m.astype(ml_dtypes.bfloat16)
    return c


CONST_SHAPES = {
    "cosH": ([128, T], F32), "sinH": ([128, T], F32), "cosR": ([128, T], F32), "sinR": ([128, T], F32),
    "cosK": ([32, T], F32), "sinK": ([32, T], F32), "cmat": ([128, 7, 128], BF16), "identf": ([128, 128], F32),
    "wmask": ([128, 6, 512], BF16),
}

IN_SHAPES = {
    "xin": [T, D], "cvec": [128, 16], "w_mod": [DEPTH, D, 6 * D], "b_mod": [DEPTH, 6 * D],
    "norm_mix": [DEPTH, D], "norm_ffn": [DEPTH, D], "wA": [DEPTH, D, 768], "wB": [DEPTH, D, 416],
    "wC": [DEPTH, D, 768], "pvec": [128, 16], "w_uq": [DEPTH, 256, 512], "wukv_k": [DEPTH, 128, 512],
    "wukv_v": [DEPTH, 128, 256], "c_sinks": [DEPTH, 6], "w_out": [DEPTH, D, D],
    "w1_dense": [1, D, DFF_D], "w3_dense": [1, D, DFF_D], "w2_dense": [1, DFF_D, D],
    "w_router": [1, D, NE], "w1_moe": [1, NE, D, DFF_E], "w3_moe": [1, NE, D, DFF_E], "w2_moe": [1, NE, DFF_E, D],
    "final_norm": [D],
}


PHASE_MARKS = []


def build(debug=False, stop_after=None, skip=()):
    del PHASE_MARKS[:]
    nc = bass.Bass("TRN2", target_bir_lowering=False)
    I = {}
    for k, shp in IN_SHAPES.items():
        I[k] = nc.dram_tensor(k, shp, F32, kind="ExternalInput").ap()
    for k, (shp, dt) in CONST_SHAPES.items():
        I[k] = nc.dram_tensor(k, shp, dt, kind="ExternalInput").ap()
    out_d = nc.dram_tensor("out", [SEQ, D], F32, kind="ExternalOutput").ap()
    skind = "ExternalOutput" if debug else "Internal"
    S = {}

    def scratch(name, shp, dt):
        S[name] = nc.dram_tensor(name, shp, dt, kind=skind).ap()

    scratch("modsc", [2, 128, 6 * D], F32)
    scratch("hT", [8, 128, T], BF16)
    scratch("mixT", [8, 128, T], BF16)
    scratch("X1", [T, D], F32)
    scratch("X2", [T, D], F32)
    scratch("X3", [T, D], F32)
    scratch("X4", [T, D], F32)

    es = contextlib.ExitStack()
    with es:
        sy = Sync(nc, es)
        V, G_, Pq = nc.vector, nc.gpsimd, nc.sync
        A_, PE = nc.scalar, nc.tensor

        uid = [0]

        def sb(st, name, shp, dt):
            uid[0] += 1
            return st.enter_context(nc.sbuf_tensor("sb%d_%s" % (uid[0], name), shp, dt))

        PSALL = es.enter_context(nc.psum_tensor("psall", [128, 8 * 512], F32))
        PS = [PSALL[:, i * 512:(i + 1) * 512] for i in range(8)]
        PSB = [sy.buf("ps%d" % i) for i in range(8)]

        cmat = sb(es, "cmat", [128, 7, 128], BF16)
        identf = sb(es, "identf", [128, 128], F32)
        epsT = sb(es, "epsT", [128, 1], F32)
        pvec = sb(es, "pvec", [128, 16], F32)
        comb = sb(es, "comb", [128, 32, 8], F32)
        B_const = sy.buf("const", dma=True)
        B_comb = [sy.buf("comb%d" % i) for i in range(32)]
        sy.dma("sp", cmat[:], I["cmat"], writes=[B_const])
        sy.dma("sp", identf[:], I["identf"], writes=[B_const])
        sy.dma("sp", pvec[:], I["pvec"], writes=[B_const])
        sy.op("pool", lambda: G_.memset(epsT[:], EPS), writes=[B_const])
        PM128, PM96, BONES, AONES, IDENT = (cmat[:, 0, :], cmat[:, 1, :], cmat[:, 2, :], cmat[:, 3, :],
                                            cmat[:, 4, :])
        PM32 = cmat[0:32, 5, 0:32]
        SEL32 = cmat[0:32, 6, :]

        DB = {}

        def db(name, idx):
            k = (name, idx)
            if k not in DB:
                DB[k] = Buf("%s_%s" % (name, idx))
            return DB[k]

        state = {"rr": 0}

        def rr(lst):
            state["rr"] += 1
            return lst[state["rr"] % len(lst)]

        def phase_mod(li):
            with contextlib.ExitStack() as st:
                cc = sb(st, "cc", [128, 16], F32)
                sil = sb(st, "sil", [128, 16], BF16)
                cl = sb(st, "cl", [128, 16, 128], BF16)
                bmod = sb(st, "bmod", [128, 6 * D], F32)
                nmx = sb(st, "nmx", [128, 2, D], F32)
                modt = [sb(st, "modt%d" % i, [128, 6 * D], F32) for i in range(2)]
                wm = [sb(st, "wm%d" % i, [128, 8, 512], BF16) for i in range(2)]
                B_cc, B_sil, B_cl = sy.buf("cc", dma=True), sy.buf("sil"), sy.buf("cl")
                B_bmod, B_nmx = sy.buf("bmod", dma=True), sy.buf("nmx", dma=True)
                B_modt = [[sy.buf("modt%d_%d" % (i, j)) for j in range(12)] for i in range(2)]
                B_modst = [sy.buf("modst%d" % i, dma=True) for i in range(2)]
                B_wm = [sy.buf("wm%d" % i, sw=True) for i in range(2)]
                sy.dma("sp", cc[:], I["cvec"], writes=[B_cc])
                sy.dma("sp", bmod[:], I["b_mod"][li].partition_broadcast(128), writes=[B_bmod])
                sy.dma("sp", nmx[:, 0, :], I["norm_mix"][li].partition_broadcast(128), writes=[B_nmx])
                sy.dma("sp", nmx[:, 1, :], I["norm_ffn"][li].partition_broadcast(128), writes=[B_nmx])
                sy.op("act", lambda: A_.activation(out=sil[:], in_=cc[:], func=AF.Silu), reads=[B_cc], writes=[B_sil])
                sy.op("dve", lambda: V.tensor_copy(out=cl[:], in_=sil[:].unsqueeze(2).to_broadcast([128, 16, 128])),
                      reads=[B_sil], writes=[B_cl])
                wsrc = I["w_mod"][li].rearrange("(k p) c -> p k c", p=128)
                for j in range(12):
                    s = j % 2
                    sy.dma("pool", wm[s][:], wsrc[:, :, j * 512:(j + 1) * 512], writes=[B_wm[s]])
                    for ty in range(2):
                        pi = (2 * j + ty) % 4
                        for k in range(8):
                            sy.op("pe", lambda k=k, ty=ty, pi=pi, s=s: PE.matmul(
                                PS[pi][:], cl[:, ty * 8 + k, :], wm[s][:, k, :], start=(k == 0), stop=(k == 7)),
                                reads=[B_cl, B_wm[s]], writes=[PSB[pi]], mark=(k == 7))
                        sy.op("dve", lambda ty=ty, pi=pi, j=j: V.tensor_tensor(
                            out=modt[ty][:, j * 512:(j + 1) * 512], in0=PS[pi][:], in1=bmod[:, j * 512:(j + 1) * 512],
                            op=ALU.add), reads=[PSB[pi], B_bmod], writes=[B_modt[ty][j]])
                for ty in range(2):
                    for (c0, ni) in ((1024, 0), (4096, 1)):
                        bl = [B_modt[ty][c0 // 512], B_modt[ty][c0 // 512 + 1]]
                        sy.op("dve", lambda ty=ty, c0=c0, ni=ni: V.scalar_tensor_tensor(
                            out=modt[ty][:, c0:c0 + 1024], in0=modt[ty][:, c0:c0 + 1024], scalar=1.0,
                            in1=nmx[:, ni, :], op0=ALU.add, op1=ALU.mult), reads=bl + [B_nmx], writes=bl)
                    sy.dma("sp", S["modsc"][ty], modt[ty][:], reads=B_modt[ty] + [B_modst[ty]],
                           writes=[db("modsc", ty)], key=B_modst[ty].dsem)
                sy.barrier()

        def load_mod(st, names):
            res = {}
            for tag, c0 in names:
                t = sb(st, "md_" + tag, [128, 2, D], F32)
                b = sy.buf("md_" + tag, dma=True)
                for ty in range(2):
                    sy.dma("sp", t[:, ty, :], S["modsc"][ty][:, c0:c0 + D], reads=[db("modsc", ty)], writes=[b])
                res[tag] = (t, b)
            return res

        def phase_norm(li, which, Xsrc, ntiles_from, with_router=False):
            with contextlib.ExitStack() as st:
                md = load_mod(st, [("G", 1024 if which == 0 else 4096), ("S", 0 if which == 0 else 3072)])
                (Gt, B_G), (St, B_S) = md["G"], md["S"]
                xt = [sb(st, "xt%d" % i, [128, D], F32) for i in range(3)]
                B_xt = [sy.buf("xt%d" % i, dma=True) for i in range(3)]
                junk = sb(st, "junk", [128, D], BF16)
                B_junk = sy.buf("junk")
                stat = [sb(st, "stat%d" % i, [128, 4], F32) for i in range(3)]
                B_stat = [sy.buf("stat%d" % i) for i in range(3)]
                tmp = [sb(st, "tmp%d" % i, [128, D], F32) for i in range(2)]
                B_tmp = [sy.buf("tmp%d" % i) for i in range(2)]
                hb = [sb(st, "hb%d" % i, [128, D], BF16) for i in range(2)]
                B_hb = [sy.buf("hb%d" % i) for i in range(2)]
                hTb = [sb(st, "hTb%d" % i, [128, 8, 512], BF16) for i in range(2)]
                B_hTb = [sy.buf("hTb%d" % i, dma=True) for i in range(2)]
                if with_router:
                    wr = sb(st, "wr", [128, 8, NE], F32)
                    B_wr = sy.buf("wr", dma=True)
                    sy.dma("sp", wr[:], I["w_router"][0].rearrange("(k p) e -> p k e", p=128), writes=[B_wr])
                    hfT = [sb(st, "hfT%d" % i, [128, 8, 128], F32) for i in range(2)]
                    B_hfT = [sy.buf("hfT%d" % i) for i in range(2)]
                    lg = [sb(st, "lg%d" % i, [128, 4, NE], F32) for i in range(2)]
                    B_lg = [sy.buf("lg%d" % i) for i in range(2)]
                    sm = [sb(st, "sm%d" % i, [128, 8], F32) for i in range(2)]
                    B_sm = [sy.buf("sm%d" % i) for i in range(2)]
                hTd = S["hT"].rearrange("k p t -> p k t")
                jobs = []
                for (c0, n) in BLOCKS:
                    tiles = [c0 // 128 + i for i in range(n // 128)]
                    tiles = [t for t in tiles if t >= ntiles_from]
                    if not tiles:
                        continue
                    hs = rr([0, 1])
                    for ti, t in enumerate(tiles):
                        jobs.append((t, ti, hs, tiles, ti == len(tiles) - 1))

                def stageL(job):
                    t, ti, hs, tiles, lastb = job
                    ty = 1 if t < 2 else 0
                    s3 = t % 3
                    s2 = t % 2
                    sy.dma("sp", xt[s3][:], Xsrc[t * 128:(t + 1) * 128, :], reads=[db(Xsrc.name, t)],
                           writes=[B_xt[s3]])

                def stageA(job):
                    t, ti, hs, tiles, lastb = job
                    ty = 1 if t < 2 else 0
                    s3 = t % 3
                    s2 = t % 2
                    sy.op("act", lambda s3=s3: A_.activation(out=junk[:], in_=xt[s3][:], func=AF.Square,
                                                             accum_out=stat[s3][:, 0:1]),
                          reads=[B_xt[s3]], writes=[B_junk, B_stat[s3]])
                    sy.op("act", lambda s3=s3: A_.activation(out=stat[s3][:, 1:2], in_=stat[s3][:, 0:1],
                                                             func=AF.Sqrt, bias=epsT[:, 0:1], scale=1.0 / D),
                          reads=[B_stat[s3], B_const], writes=[B_stat[s3]])
                    sy.op("dve", lambda s3=s3: V.reciprocal(out=stat[s3][:, 2:3], in_=stat[s3][:, 1:2]),
                          reads=[B_stat[s3]], writes=[B_stat[s3]])
                    sy.op("dve", lambda s3=s3, s2=s2, ty=ty: V.scalar_tensor_tensor(
                        out=tmp[s2][:], in0=xt[s3][:], scalar=stat[s3][:, 2:3], in1=Gt[:, ty, :],
                        op0=ALU.mult, op1=ALU.mult), reads=[B_xt[s3], B_stat[s3], B_G], writes=[B_tmp[s2]])
                    if with_router:
                        sy.op("pool", lambda s2=s2, ty=ty: G_.tensor_tensor(
                            out=tmp[s2][:], in0=tmp[s2][:], in1=St[:, ty, :], op=ALU.add),
                            reads=[B_tmp[s2], B_S], writes=[B_tmp[s2]])
                        sy.op("act", lambda s2=s2: A_.copy(out=hb[s2][:], in_=tmp[s2][:]),
                              reads=[B_tmp[s2]], writes=[B_hb[s2]])
                    else:
                        sy.op("pool", lambda s2=s2, ty=ty: G_.tensor_tensor(
                            out=hb[s2][:], in0=tmp[s2][:], in1=St[:, ty, :], op=ALU.add),
                            reads=[B_tmp[s2], B_S], writes=[B_hb[s2]])

                def stageB(job):
                    t, ti, hs, tiles, lastb = job
                    ty = 1 if t < 2 else 0
                    s3 = t % 3
                    s2 = t % 2
                    pi = t % 2
                    pst = PS[pi][:].bitcast(BF16)
                    for k in range(8):
                        sy.op("pe", lambda k=k, s2=s2, pst=pst: PE.transpose(
                            pst[:, k * 128:(k + 1) * 128], hb[s2][:, k * 128:(k + 1) * 128], IDENT),
                            reads=[B_hb[s2], B_const], writes=[PSB[pi]], mark=(k == 7))
                    sy.op("act", lambda ti=ti, hs=hs, pst=pst: A_.copy(
                        out=hTb[hs][:, :, ti * 128:(ti + 1) * 128],
                        in_=pst.rearrange("p (k t) -> p k t", k=8)),
                        reads=[PSB[pi]], writes=[B_hTb[hs]])
                    if with_router:
                        j = t - 2
                        for half in range(2):
                            pj = 2 + half
                            for k4 in range(4):
                                k = half * 4 + k4
                                sy.op("pe", lambda k=k, k4=k4, s2=s2, pj=pj: PE.transpose(
                                    PS[pj][:, k4 * 128:(k4 + 1) * 128], tmp[s2][:, k * 128:(k + 1) * 128],
                                    identf[:]), reads=[B_tmp[s2], B_const], writes=[PSB[pj]], mark=(k4 == 3))
                            sy.op("dve", lambda half=half, s2=s2, pj=pj: V.tensor_copy(
                                out=hfT[s2][:, half * 4:(half + 1) * 4, :],
                                in_=PS[pj][:].rearrange("p (k t) -> p k t", k=4)),
                                reads=[PSB[pj]], writes=[B_hfT[s2]])
                        for k in range(8):
                            sy.op("pe", lambda k=k, s2=s2: PE.matmul(
                                PS[4][:, 0:NE], hfT[s2][:, k, :], wr[:, k, :], start=(k == 0), stop=(k == 7)),
                                reads=[B_hfT[s2], B_wr], writes=[PSB[4]], mark=(k == 7))
                        L = lg[s2]
                        smt = sm[s2]
                        bl = [B_lg[s2], B_sm[s2]]
                        sy.op("dve", lambda L=L: V.tensor_copy(out=L[:, 0, :], in_=PS[4][:, 0:NE]),
                              reads=[PSB[4]], writes=bl)
                        sy.op("dve", lambda L=L, smt=smt: V.tensor_reduce(
                            out=smt[:, 0:1], in_=L[:, 0, :], axis=AX.X, op=ALU.max), reads=bl, writes=bl)
                        sy.op("dve", lambda L=L, smt=smt: V.tensor_scalar(
                            out=L[:, 1, :], in0=L[:, 0, :], scalar1=smt[:, 0:1], scalar2=None,
                            op0=ALU.is_equal), reads=bl, writes=bl)
                        sy.op("dve", lambda L=L: V.scalar_tensor_tensor(
                            out=L[:, 2, :], in0=L[:, 1, :], scalar=-1e30, in1=L[:, 0, :],
                            op0=ALU.mult, op1=ALU.add), reads=bl, writes=bl)
                        sy.op("dve", lambda L=L, smt=smt: V.tensor_reduce(
                            out=smt[:, 1:2], in_=L[:, 2, :], axis=AX.X, op=ALU.max), reads=bl, writes=bl)
                        sy.op("dve", lambda L=L, smt=smt: V.tensor_scalar(
                            out=L[:, 3, :], in0=L[:, 2, :], scalar1=smt[:, 1:2], scalar2=None,
                            op0=ALU.is_equal), reads=bl, writes=bl)
                        sy.op("dve", lambda smt=smt: V.tensor_tensor(
                            out=smt[:, 2:3], in0=smt[:, 1:2], in1=smt[:, 0:1], op=ALU.subtract),
                            reads=bl, writes=bl)
                        sy.op("act", lambda smt=smt: A_.activation(out=smt[:, 3:4], in_=smt[:, 2:3], func=AF.Exp),
                              reads=bl, writes=bl)
                        sy.op("dve", lambda smt=smt: V.tensor_scalar(
                            out=smt[:, 4:5], in0=smt[:, 3:4], scalar1=1.0, scalar2=None, op0=ALU.add),
                            reads=bl, writes=bl)
                        sy.op("dve", lambda smt=smt: V.reciprocal(out=smt[:, 5:6], in_=smt[:, 4:5]),
                              reads=bl, writes=bl)
                        sy.op("dve", lambda smt=smt: V.tensor_tensor(
                            out=smt[:, 6:7], in0=smt[:, 3:4], in1=smt[:, 5:6], op=ALU.mult),
                            reads=bl, writes=bl)
                        sy.op("dve", lambda L=L, smt=smt: V.tensor_scalar(
                            out=L[:, 1, :], in0=L[:, 1, :], scalar1=smt[:, 5:6], scalar2=None, op0=ALU.mult),
                            reads=bl, writes=bl)
                        sy.op("dve", lambda L=L, smt=smt, j=j: V.scalar_tensor_tensor(
                            out=comb[:, j, :], in0=L[:, 3, :], scalar=smt[:, 6:7], in1=L[:, 1, :],
                            op0=ALU.mult, op1=ALU.add), reads=bl, writes=[B_comb[j]])

                    if lastb:
                        tb0 = tiles[0] * 128
                        nn = len(tiles) * 128
                        sy.dma("sp", hTd[:, :, tb0:tb0 + nn], hTb[hs][:, :, 0:nn], reads=[B_hTb[hs]],
                               writes=[db("hT", t_) for t_ in tiles])

                nj = len(jobs)
                for q in range(min(3, nj)):
                    stageL(jobs[q])
                stageA(jobs[0])
                for q in range(nj):
                    if q + 1 < nj:
                        stageA(jobs[q + 1])
                    stageB(jobs[q])
                    if q + 3 < nj:
                        stageL(jobs[q + 3])
                sy.barrier()

        def phase_mixer(li, mx, with_ctx):
            gl = (mx in "AC")
            with contextlib.ExitStack() as st:
                if gl:
                    wsrc = I["wA" if mx == "A" else "wC"][li]
                    wcols = 768
                    qT = sb(st, "qT", [128, 3, T], BF16)
                    kT = sb(st, "kT", [128, 2, T], BF16)
                    Va = sb(st, "Va", [128, NT, 2, 192], BF16)
                else:
                    wsrc = I["wB"][li]
                    wcols = 416
                    qT = sb(st, "qT", [128, 4, T], BF16)
                    kT = sb(st, "kT", [128, 4, T], BF16)
                    Va = sb(st, "Va", [128, NT, 4, 128], BF16)
                    wuq = sb(st, "wuq", [128, 2, 512], BF16)
                    wuk = sb(st, "wuk", [128, 512], BF16)
                    wuv = sb(st, "wuv", [128, 256], BF16)
                win = sb(st, "win", [128, 8, wcols], BF16)
                B_w = sy.buf("win", sw=True)
                sy.dma("pool", win[:], wsrc.rearrange("(k p) c -> p k c", p=128), writes=[B_w])
                if not gl:
                    sy.dma("pool", wuq[:], I["w_uq"][li].rearrange("(k p) c -> p k c", p=128), writes=[B_w])
                    sy.dma("pool", wuk[:], I["wukv_k"][li], writes=[B_w])
                    sy.dma("pool", wuv[:], I["wukv_v"][li], writes=[B_w])
                B_q = {}
                B_k = {}
                B_v = [sy.buf("v%d" % t) for t in range(NT)]
                B_vones = sy.buf("vones")

                def bq(key):
                    if key not in B_q:
                        B_q[key] = sy.buf("q%s" % (key,))
                    return B_q[key]

                def bk(key):
                    if key not in B_k:
                        B_k[key] = sy.buf("k%s" % (key,))
                    return B_k[key]

                if gl:
                    sy.op("pool", lambda: G_.memset(Va[:, :, :, 0:64], 1.0), writes=[B_vones])
                    sy.op("pool", lambda: G_.memset(Va[:, :, :, 128:192], 1.0), writes=[B_vones])
                else:
                    for h in range(4):
                        o = 64 if h % 2 == 0 else 0
                        sy.op("pool", lambda h=h, o=o: G_.memset(Va[:, :, h, o:o + 64], 1.0), writes=[B_vones])
                if mx == "C":
                    wmask = sb(st, "wmask", [128, 6, 512], BF16)
                    B_wmask = sy.buf("wmask", dma=True)
                    sy.dma("sp", wmask[:], I["wmask"], writes=[B_wmask])
                    sk = sb(st, "sk", [128, 8], F32)
                    B_sk = sy.buf("sk", dma=True)
                    sy.dma("sp", sk[:, 0:6], I["c_sinks"][li].partition_broadcast(128), writes=[B_sk])
                    sy.op("act", lambda: A_.activation(out=sk[:, 0:6], in_=sk[:, 0:6], func=AF.Exp),
                          reads=[B_sk], writes=[B_sk])

                hTb = [sb(st, "hTb%d" % i, [128, 8, 512], BF16) for i in range(2)]
                B_hTb = [sy.buf("hTb%d" % i, dma=True) for i in range(2)]
                cs = [sb(st, "cs%d" % i, [128, 2, 512], F32) for i in range(2)]
                B_cs = [sy.buf("cs%d" % i, dma=True) for i in range(2)]
                if not gl:
                    csK = [sb(st, "csK%d" % i, [32, 2, 512], F32) for i in range(2)]
                NS = 4 if gl else 3
                ub = [sb(st, "ub%d" % i, [128, 512], BF16) for i in range(NS)]
                B_ub = [sy.buf("ub%d" % i) for i in range(NS)]
                sq = [sb(st, "sq%d" % i, [128, 512], BF16) for i in range(3)]
                B_sq = [sy.buf("sq%d" % i) for i in range(3)]
                f1 = [sb(st, "f1_%d" % i, [128, 512], F32) for i in range(NS)]
                B_f1 = [sy.buf("f1_%d" % i) for i in range(NS)]
                f2 = [sb(st, "f2_%d" % i, [128, 512], F32) for i in range(NS)]
                B_f2 = [sy.buf("f2_%d" % i) for i in range(NS)]
                rs = [sb(st, "rs%d" % i, [128, 512], F32) for i in range(NS)]
                B_rs = [sy.buf("rs%d" % i) for i in range(NS)]
                if not gl:
                    cqf = [sb(st, "cqf%d" % i, [128, 512], F32) for i in range(3)]
                    B_cqf = [sy.buf("cqf%d" % i) for i in range(3)]
                    cqn = [sb(st, "cqn%d" % i, [128, 512], BF16) for i in range(3)]
                    B_cqn = [sy.buf("cqn%d" % i) for i in range(3)]
                    krf = sb(st, "krf", [32, 512], BF16)
                    B_krf = sy.buf("krf")

                hTd = S["hT"].rearrange("k p t -> p k t")
                cnt = {"s": 0}

                def nxt(n):
                    cnt["s"] += 1
                    return cnt["s"] % n

                lc = li * 5

                def rope_store(pi, n, dst_ap, dstbuf, cs_t, B_cst, rows, pm, rstd=None, gain_col=None):
                    s = nxt(NS)
                    pr = 4 + nxt(2)
                    if gain_col is not None:
                        sy.op("act", lambda: A_.activation(out=ub[s][0:rows, 0:n], in_=PS[pi][0:rows, 0:n],
                                                           func=AF.Copy, scale=pvec[0:rows, gain_col:gain_col + 1]),
                              reads=[PSB[pi], B_const], writes=[B_ub[s]])
                    else:
                        sy.op("act", lambda: A_.copy(out=ub[s][0:rows, 0:n], in_=PS[pi][0:rows, 0:n]),
                              reads=[PSB[pi]], writes=[B_ub[s]])
                    sy.op("pe", lambda: PE.matmul(PS[pr][0:rows, 0:n], pm, ub[s][0:rows, 0:n], start=True, stop=True),
                          reads=[B_ub[s], B_const], writes=[PSB[pr]])
                    if gain_col is not None:
                        sy.op("dve", lambda: V.tensor_tensor(out=f1[s][0:rows, 0:n], in0=ub[s][0:rows, 0:n],
                                                             in1=cs_t[0:rows, 0, 0:n], op=ALU.mult),
                              reads=[B_ub[s], B_cst], writes=[B_f1[s]])
                    else:
                        sy.op("dve", lambda: V.tensor_tensor(out=f1[s][0:rows, 0:n], in0=ub[s][0:rows, 0:n],
                                                             in1=cs_t[0:rows, 0, 0:n], op=ALU.mult),
                              reads=[B_ub[s], B_cst], writes=[B_f1[s]])
                    sy.op("dve", lambda: V.tensor_tensor(out=f2[s][0:rows, 0:n], in0=PS[pr][0:rows, 0:n],
                                                         in1=cs_t[0:rows, 1, 0:n], op=ALU.mult),
                          reads=[PSB[pr], B_cst], writes=[B_f2[s]])
                    if rstd is None:
                        sy.op("pool", lambda: G_.tensor_tensor(out=dst_ap, in0=f1[s][0:rows, 0:n],
                                                               in1=f2[s][0:rows, 0:n], op=ALU.add),
                              reads=[B_f1[s], B_f2[s]], writes=[dstbuf])
                    else:
                        rt, B_rt = rstd
                        sy.op("pool", lambda: G_.tensor_tensor(out=f1[s][0:rows, 0:n], in0=f1[s][0:rows, 0:n],
                                                               in1=f2[s][0:rows, 0:n], op=ALU.add),
                              reads=[B_f1[s], B_f2[s]], writes=[B_f1[s]])
                        sy.op("dve", lambda: V.tensor_tensor(out=dst_ap, in0=f1[s][0:rows, 0:n],
                                                             in1=rt[0:rows, 0:n], op=ALU.mult),
                              reads=[B_f1[s], B_rt], writes=[dstbuf])

                def rstd_from(pss, n, scale, rows=128):
                    s = nxt(NS)
                    sy.op("act", lambda: A_.activation(out=rs[s][0:rows, 0:n], in_=PS[pss][0:rows, 0:n], func=AF.Ln,
                                                       bias=epsT[0:rows, 0:1], scale=scale),
                          reads=[PSB[pss], B_const], writes=[B_rs[s]])
                    sy.op("act", lambda: A_.activation(out=rs[s][0:rows, 0:n], in_=rs[s][0:rows, 0:n], func=AF.Exp,
                                                       scale=-0.5),
                          reads=[B_rs[s]], writes=[B_rs[s]])
                    return rs[s], B_rs[s]

                def proj(pi, c0, m, hs, n):
                    for k in range(8):
                        sy.op("pe", lambda k=k: PE.matmul(PS[pi][0:m, 0:n], win[:, k, c0:c0 + m], hTb[hs][:, k, 0:n],
                                                          start=(k == 0), stop=(k == 7)),
                              reads=[B_w, B_hTb[hs]], writes=[PSB[pi]], mark=(k == 7))

                for bi, (c0, n) in enumerate(BLOCKS):
                    hs = bi % 2
                    tiles = [c0 // 128 + i for i in range(n // 128)]
                    sy.dma("sp", hTb[hs][:, :, 0:n], hTd[:, :, c0:c0 + n], reads=[db("hT", t) for t in tiles],
                           writes=[B_hTb[hs]])
                    isctx = (bi == 0)
                    if gl:
                        sy.dma("sp", cs[hs][:, 0, 0:n], I["cosH"][:, c0:c0 + n], writes=[B_cs[hs]])
                        sy.dma("sp", cs[hs][:, 1, 0:n], I["sinH"][:, c0:c0 + n], writes=[B_cs[hs]])
                        for ch in range(5):
                            if ch < 3 and isctx and not with_ctx:
                                continue
                            pi = nxt(2)
                            proj(pi, ch * 128, 128, hs, n)
                            if ch < 3:
                                dst = qT[:, ch, c0:c0 + n]
                                dbuf = bq((ch, bi))
                            else:
                                dst = kT[:, ch - 3, c0:c0 + n]
                                dbuf = bk((ch - 3, bi))
                            if mx == "A":
                                gcol = lc + (0 if ch < 3 else 1)
                                s3 = nxt(3)
                                sy.op("act", lambda pi=pi, s3=s3: A_.activation(
                                    out=sq[s3][:, 0:n], in_=PS[pi][:, 0:n], func=AF.Square),
                                    reads=[PSB[pi]], writes=[B_sq[s3]])
                                pss = 6 + nxt(2)
                                sy.op("pe", lambda s3=s3, pss=pss: PE.matmul(PS[pss][:, 0:n], BONES, sq[s3][:, 0:n],
                                                                             start=True, stop=True),
                                      reads=[B_sq[s3], B_const], writes=[PSB[pss]])
                                rst = rstd_from(pss, n, 1.0 / HD)
                                rope_store(pi, n, dst, dbuf, cs[hs], B_cs[hs], 128, PM128, rstd=rst, gain_col=gcol)
                            else:
                                rope_store(pi, n, dst, dbuf, cs[hs], B_cs[hs], 128, PM128)
                        for ti, t in enumerate(tiles):
                            pv = 2 + nxt(2)
                            for k in range(8):
                                sy.op("pe", lambda k=k, ti=ti, pv=pv: PE.matmul(
                                    PS[pv][:, 0:128], hTb[hs][:, k, ti * 128:(ti + 1) * 128], win[:, k, 640:768],
                                    start=(k == 0), stop=(k == 7)), reads=[B_w, B_hTb[hs]], writes=[PSB[pv]],
                                    mark=(k == 7))
                            sy.op("dve", lambda t=t, pv=pv: V.tensor_copy(
                                out=Va[:, t, :, 64:128], in_=PS[pv][:, 0:128].rearrange("p (h d) -> p h d", h=2)),
                                reads=[PSB[pv], B_vones], writes=[B_v[t]])
                    else:
                        sy.dma("sp", cs[hs][:, 0, 0:n], I["cosR"][:, c0:c0 + n], writes=[B_cs[hs]])
                        sy.dma("sp", cs[hs][:, 1, 0:n], I["sinR"][:, c0:c0 + n], writes=[B_cs[hs]])
                        sy.dma("sp", csK[hs][:, 0, 0:n], I["cosK"][:, c0:c0 + n], writes=[B_cs[hs]])
                        sy.dma("sp", csK[hs][:, 1, 0:n], I["sinK"][:, c0:c0 + n], writes=[B_cs[hs]])
                        need_q = (not isctx) or with_ctx
                        pss_q = 6
                        pss_kv = 7
                        for ch in range(3):
                            if ch < 2 and not need_q:
                                continue
                            pi = nxt(2)
                            proj(pi, ch * 128, 128, hs, n)
                            s3 = nxt(3)
                            sy.op("act", lambda pi=pi, s3=s3: A_.activation(
                                out=sq[s3][:, 0:n], in_=PS[pi][:, 0:n], func=AF.Square),
                                reads=[PSB[pi]], writes=[B_sq[s3]])
                            gcol = lc + 2 + ch
                            sy.op("act", lambda pi=pi, ch=ch, gcol=gcol: A_.activation(
                                out=cqf[ch][:, 0:n], in_=PS[pi][:, 0:n], func=AF.Copy,
                                scale=pvec[:, gcol:gcol + 1]), reads=[PSB[pi], B_const], writes=[B_cqf[ch]])
                            if ch < 2:
                                sy.op("pe", lambda s3=s3, ch=ch: PE.matmul(PS[pss_q][:, 0:n], AONES, sq[s3][:, 0:n],
                                                                           start=(ch == 0), stop=(ch == 1)),
                                      reads=[B_sq[s3], B_const], writes=[PSB[pss_q]], mark=(ch == 1))
                            else:
                                sy.op("pe", lambda s3=s3: PE.matmul(PS[pss_kv][:, 0:n], AONES, sq[s3][:, 0:n],
                                                                    start=True, stop=True),
                                      reads=[B_sq[s3], B_const], writes=[PSB[pss_kv]])
                        if need_q:
                            rt, B_rt = rstd_from(pss_q, n, 1.0 / 256)
                            for ch in range(2):
                                sy.op("dve", lambda ch=ch, rt=rt: V.tensor_tensor(
                                    out=cqn[ch][:, 0:n], in0=cqf[ch][:, 0:n], in1=rt[:, 0:n], op=ALU.mult),
                                    reads=[B_cqf[ch], B_rt], writes=[B_cqn[ch]])
                        rt, B_rt = rstd_from(pss_kv, n, 1.0 / 128)
                        sy.op("dve", lambda rt=rt: V.tensor_tensor(
                            out=cqn[2][:, 0:n], in0=cqf[2][:, 0:n], in1=rt[:, 0:n], op=ALU.mult),
                            reads=[B_cqf[2], B_rt], writes=[B_cqn[2]])
                        pi = nxt(2)
                        proj(pi, 384, 32, hs, n)
                        rope_store(pi, n, krf[:, 0:n], B_krf, csK[hs], B_cs[hs], 32, PM32)
                        if need_q:
                            for h in range(4):
                                pi = nxt(2)
                                for c2 in range(2):
                                    sy.op("pe", lambda c2=c2, h=h, pi=pi: PE.matmul(
                                        PS[pi][:, 0:n], wuq[:, c2, h * 128:(h + 1) * 128], cqn[c2][:, 0:n],
                                        start=(c2 == 0), stop=(c2 == 1)), reads=[B_w, B_cqn[c2]], writes=[PSB[pi]],
                                        mark=(c2 == 1))
                                rope_store(pi, n, qT[:, h, c0:c0 + n], bq((h, bi)), cs[hs], B_cs[hs], 128, PM96)
                        for h in range(4):
                            pi = nxt(2)
                            sy.op("pe", lambda h=h, pi=pi: PE.matmul(
                                PS[pi][:, 0:n], wuk[:, h * 128:(h + 1) * 128], cqn[2][:, 0:n], start=True,
                                stop=False), reads=[B_w, B_cqn[2]], writes=[PSB[pi]], mark=False)
                            sy.op("pe", lambda pi=pi: PE.matmul(
                                PS[pi][:, 0:n], SEL32, krf[:, 0:n], start=False, stop=True),
                                reads=[B_const, B_krf], writes=[PSB[pi]])
                            sy.op("act", lambda h=h, pi=pi: A_.copy(out=kT[:, h, c0:c0 + n], in_=PS[pi][:, 0:n]),
                                  reads=[PSB[pi]], writes=[bk((h, bi))])
                        for ti, t in enumerate(tiles):
                            pv = 2 + nxt(2)
                            sy.op("pe", lambda ti=ti, pv=pv: PE.matmul(
                                PS[pv][:, 0:256], cqn[2][:, ti * 128:(ti + 1) * 128], wuv[:], start=True, stop=True),
                                reads=[B_w, B_cqn[2]], writes=[PSB[pv]])
                            for h in range(4):
                                o = 0 if h % 2 == 0 else 64
                                sy.op("dve", lambda t=t, pv=pv, h=h, o=o: V.tensor_copy(
                                    out=Va[:, t, h, o:o + 64], in_=PS[pv][:, h * 64:(h + 1) * 64]),
                                    reads=[PSB[pv], B_vones], writes=[B_v[t]])

                nheads = 6 if gl else 4
                scale = (HD ** -0.5) if gl else (96 ** -0.5)
                GS = 3
                pt = [sb(st, "pt%d" % i, [128, GS, 512], BF16) for i in range(2)]
                B_pt = [sy.buf("pt%d" % i) for i in range(2)]
                rec = [sb(st, "rec%d" % i, [128, 512], F32) for i in range(2)]
                B_rec = [sy.buf("rec%d" % i) for i in range(2)]
                mo = [sb(st, "mo%d" % i, [128, 512], BF16) for i in range(2)]
                B_mo = [sy.buf("mo%d" % i, dma=True) for i in range(2)]
                OPS = [6, 7]
                qblocks = list(range(1, 9)) + ([0] if with_ctx else [])
                def make_item(qb, h, it):
                    c0, n = BLOCKS[qb]
                    if gl:
                        kvh = h // 3
                        par = h % 2
                        base = par * 64
                        K = 64
                        q_ap = qT[base:base + 64, h // 2, c0:c0 + n]
                        q_buf = bq((h // 2, qb))
                        kidx = 0 if (kvh == par) else 1
                        mixchunk = (0 if mx == "A" else 5) + h // 2
                    else:
                        kvh = h
                        par = h % 2
                        base = 0
                        K = 128
                        q_ap = qT[:, h, c0:c0 + n]
                        q_buf = bq((h, qb))
                        kidx = h
                        mixchunk = 3 + h // 2
                    if qb == 0:
                        kl = [(0, None), (1, None)]
                    elif mx == "C":
                        l = qb - 1
                        kl = [(0, None), (1, None)]
                        for ci, cc in enumerate(range(-1, 5)):
                            j = 4 * l + cc
                            if 0 <= j < 32:
                                kl.append((2 + j, ci))
                    else:
                        kl = [(t, None) for t in range(NT)]
                    po = OPS[it % 2]
                    nk = len(kl)
                    sink = (mx == "C")

                    groups = [kl[i:i + GS] for i in range(0, nk, GS)]
                    ng = len(groups)

                    def qkg(j, sl_):
                        for ci, (t, _) in enumerate(groups[j]):
                            bank = GS * sl_ + ci
                            kb = 0 if t < 2 else 1 + (t - 2) // 4
                            k_ap = kT[base:base + K, kidx, t * 128:(t + 1) * 128]
                            sy.op("pe", lambda bank=bank, k_ap=k_ap: PE.matmul(
                                PS[bank][:, 0:n], k_ap, q_ap, start=True, stop=True),
                                reads=[bk((kidx, kb)), q_buf], writes=[PSB[bank]],
                                mark=(ci == len(groups[j]) - 1))

                    def pvg(j, sl_):
                        g = len(groups[j])
                        src = PSALL[:, GS * sl_ * 512:(GS * sl_ + g) * 512].rearrange("p (c n) -> p c n", c=g)
                        sy.op("act", lambda: A_.activation(out=pt[sl_][:, 0:g, 0:n], in_=src[:, :, 0:n],
                                                           func=AF.Exp, scale=scale),
                              reads=[PSB[GS * sl_ + ci] for ci in range(g)], writes=[B_pt[sl_]])
                        for ci, (t, mid) in enumerate(groups[j]):
                            if mid is not None:
                                sy.op("pool", lambda ci=ci, mid=mid: G_.tensor_tensor(
                                    out=pt[sl_][:, ci, 0:n], in0=pt[sl_][:, ci, 0:n], in1=wmask[:, mid, 0:n],
                                    op=ALU.mult), reads=[B_pt[sl_], B_wmask], writes=[B_pt[sl_]])
                        for ci, (t, mid) in enumerate(groups[j]):
                            if gl:
                                v_ap = Va[:, t, kvh, 64:192] if par == 0 else Va[:, t, kvh, 0:128]
                            else:
                                v_ap = Va[:, t, h, :]
                            first_ = (j == 0 and ci == 0)
                            last_ = (j == ng - 1 and ci == g - 1)
                            sy.op("pe", lambda ci=ci, v_ap=v_ap, first_=first_, last_=last_: PE.matmul(
                                PS[po][:, 0:n], v_ap, pt[sl_][:, ci, 0:n], start=first_, stop=last_),
                                reads=[B_v[t], B_vones, B_pt[sl_]], writes=[PSB[po]], mark=(ci == g - 1))

                    def epi():
                        orow = slice(0, 64) if par == 0 else slice(64, 128)
                        srow = slice(64, 128) if par == 0 else slice(0, 64)
                        s2 = it % 2
                        if sink:
                            sy.op("dve", lambda: V.tensor_copy(out=rec[s2][orow, 0:n], in_=PS[po][srow, 0:n]),
                                  reads=[PSB[po]], writes=[B_rec[s2]])
                            sy.op("dve", lambda: V.tensor_scalar(out=rec[s2][orow, 0:n], in0=rec[s2][orow, 0:n],
                                                                 scalar1=sk[orow, h:h + 1], scalar2=None, op0=ALU.add),
                                  reads=[B_rec[s2], B_sk], writes=[B_rec[s2]])
                            sy.op("dve", lambda: V.reciprocal(out=rec[s2][orow, 0:n], in_=rec[s2][orow, 0:n]),
                                  reads=[B_rec[s2]], writes=[B_rec[s2]])
                        else:
                            sy.op("dve", lambda: V.reciprocal(out=rec[s2][orow, 0:n], in_=PS[po][srow, 0:n]),
                                  reads=[PSB[po]], writes=[B_rec[s2]])
                        sy.op("dve", lambda: V.tensor_tensor(out=mo[s2][orow, 0:n], in0=PS[po][orow, 0:n],
                                                             in1=rec[s2][orow, 0:n], op=ALU.mult),
                              reads=[PSB[po], B_rec[s2]], writes=[B_mo[s2]])
                        sy.dma("sp", S["mixT"][mixchunk, orow, c0:c0 + n], mo[s2][orow, 0:n], reads=[B_mo[s2]],
                               writes=[db("mixT", (mixchunk, par, qb))])

                    return (ng, qkg, pvg, epi)

                items = [make_item(qb, h, idx) for idx, (qb, h) in
                         enumerate([(qb, h) for qb in qblocks for h in range(nheads)])]
                seq = [(im, j) for im in items for j in range(im[0])]
                if seq:
                    seq[0][0][1](seq[0][1], 0)
                for x, (im, j) in enumerate(seq):
                    if x + 1 < len(seq):
                        nim, nj = seq[x + 1]
                        nim[1](nj, (x + 1) % 2)
                    im[2](j, x % 2)
                    if j == im[0] - 1:
                        im[3]()
                sy.barrier()

        def phase_wout(li, Xsrc, Xdst, tile_from):
            with contextlib.ExitStack() as st:
                md = load_mod(st, [("GT", 2048)])
                GTt, B_GT = md["GT"]
                wo = sb(st, "wo", [128, 8, D], BF16)
                B_wo = sy.buf("wo", sw=True)
                sy.dma("pool", wo[:], I["w_out"][li].rearrange("(k p) c -> p k c", p=128), writes=[B_wo])
                mT = [sb(st, "mT%d" % i, [128, 8, 512], BF16) for i in range(2)]
                B_mT = [sy.buf("mT%d" % i, dma=True) for i in range(2)]
                xt = [sb(st, "xt%d" % i, [128, D], F32) for i in range(3)]
                B_xt = [sy.buf("xt%d" % i, dma=True) for i in range(3)]
                tm = [sb(st, "tm%d" % i, [128, D], F32) for i in range(2)]
                B_tm = [sy.buf("tm%d" % i) for i in range(2)]
                mTd = S["mixT"].rearrange("k p t -> p k t")
                blks = []
                jobs = []
                for bi, (c0, n) in enumerate(BLOCKS):
                    tiles = [c0 // 128 + i for i in range(n // 128)]
                    if tiles[0] < tile_from:
                        continue
                    blks.append((bi, c0, n))
                    for ti, t in enumerate(tiles):
                        jobs.append((len(blks) - 1, ti, t))

                def load_blk(bx):
                    bi, c0, n = blks[bx]
                    hs = bx % 2
                    rd = [db("mixT", (ch, par, bi)) for ch in range(8) for par in range(2)]
                    sy.dma("sp", mT[hs][:, :, 0:n], mTd[:, :, c0:c0 + n], reads=rd, writes=[B_mT[hs]])

                def load_x(job):
                    bx, ti, t = job
                    s3 = t % 3
                    sy.dma("sp", xt[s3][:], Xsrc[t * 128:(t + 1) * 128, :], reads=[db(Xsrc.name, t)],
                           writes=[B_xt[s3]])

                def compute(job):
                    bx, ti, t = job
                    hs = bx % 2
                    ty = 1 if t < 2 else 0
                    s3 = t % 3
                    s2 = t % 2
                    for dh in range(2):
                        pi = (2 * t + dh) % 4
                        for k in range(8):
                            sy.op("pe", lambda k=k, pi=pi, dh=dh: PE.matmul(
                                PS[pi][:], mT[hs][:, k, ti * 128:(ti + 1) * 128], wo[:, k, dh * 512:(dh + 1) * 512],
                                start=(k == 0), stop=(k == 7)), reads=[B_mT[hs], B_wo], writes=[PSB[pi]],
                                mark=(k == 7))
                        sy.op("dve", lambda pi=pi, dh=dh, ty=ty: V.tensor_tensor(
                            out=tm[s2][:, dh * 512:(dh + 1) * 512], in0=PS[pi][:],
                            in1=GTt[:, ty, dh * 512:(dh + 1) * 512], op=ALU.mult),
                            reads=[PSB[pi], B_GT], writes=[B_tm[s2]])
                    sy.op("pool", lambda s3=s3, s2=s2: G_.tensor_tensor(out=xt[s3][:], in0=xt[s3][:], in1=tm[s2][:],
                                                                        op=ALU.add),
                          reads=[B_xt[s3], B_tm[s2]], writes=[B_xt[s3]])
                    sy.dma("sp", Xdst[t * 128:(t + 1) * 128, :], xt[s3][:], reads=[B_xt[s3]],
                           writes=[db(Xdst.name, t)])

                nj = len(jobs)
                load_blk(0)
                for q in range(min(3, nj)):
                    load_x(jobs[q])
                for q in range(nj):
                    bx, ti, t = jobs[q]
                    if ti == 0 and bx + 1 < len(blks):
                        load_blk(bx + 1)
                    compute(jobs[q])
                    if q + 3 < nj:
                        load_x(jobs[q + 3])
                sy.barrier()

        def phase_ffn(li, Xsrc, Xdst, tile_from, moe, final):
            ntok_tiles = NT - tile_from
            half = ntok_tiles // 2
            supers = [(tile_from, half), (tile_from + half, ntok_tiles - half)]
            dff = DFF_E if moe else DFF_D
            nfc = dff // 128
            groups = [(g0, min(4, nfc - g0)) for g0 in range(0, nfc, 4)]
            nexp = NE if moe else 1
            with contextlib.ExitStack() as st:
                md = load_mod(st, [("GT", 5120)])
                GTt, B_GT = md["GT"]
                maxst = max(s[1] for s in supers)
                h2 = sb(st, "h2", [128, 8, maxst * 128], BF16)
                yacc = sb(st, "yacc", [128, maxst, D], F32)
                B_h2 = sy.buf("h2", dma=True)
                B_y = [sy.buf("y%d" % i) for i in range(maxst)]
                W1 = [sb(st, "W1_%d" % i, [128, 8, 512], BF16) for i in range(2)]
                W3 = [sb(st, "W3_%d" % i, [128, 8, 512], BF16) for i in range(2)]
                W2 = [sb(st, "W2_%d" % i, [128, 4, D], BF16) for i in range(2)]
                B_W = [sy.buf("W%d" % i, sw=True) for i in range(2)]
                sa = [sb(st, "sa%d" % i, [128, 512], BF16) for i in range(2)]
                B_sa = [sy.buf("sa%d" % i) for i in range(2)]
                gg = [sb(st, "gg%d" % i, [128, 4, 512], BF16) for i in range(2)]
                B_gg = [[sy.buf("gg%d_%d" % (i, f)) for f in range(4)] for i in range(2)]
                xt = [sb(st, "xt%d" % i, [128, D], F32) for i in range(2)]
                B_xt = [sy.buf("xt%d" % i, dma=True) for i in range(2)]
                if final:
                    fn = sb(st, "fn", [128, D], F32)
                    B_fn = sy.buf("fn", dma=True)
                    sy.dma("sp", fn[:], I["final_norm"].partition_broadcast(128), writes=[B_fn])
                    junk = sb(st, "junk", [128, D], BF16)
                    B_junk = sy.buf("junk")
                    stat = [sb(st, "stat%d" % i, [128, 4], F32) for i in range(2)]
                    B_stat = [sy.buf("stat%d" % i) for i in range(2)]
                hTd = S["hT"].rearrange("k p t -> p k t")
                gi = 0
                def load_h2(si, q):
                    t0_, ntl_ = supers[si]
                    sy.dma(q, h2[:, :, 0:ntl_ * 128], hTd[:, :, t0_ * 128:(t0_ + ntl_) * 128],
                           reads=[db("hT", t) for t in range(t0_, t0_ + ntl_)], writes=[B_h2])

                load_h2(0, "sp")
                for si, (t0, ntl) in enumerate(supers):
                    ntok = ntl * 128
                    tbs = [(o, min(512, ntok - o)) for o in range(0, ntok, 512)]
                    first = True
                    for e in range(nexp):
                        if moe:
                            w1s = I["w1_moe"][0, e]
                            w3s = I["w3_moe"][0, e]
                            w2s = I["w2_moe"][0, e]
                        else:
                            w1s = I["w1_dense"][0]
                            w3s = I["w3_dense"][0]
                            w2s = I["w2_dense"][0]
                        w1r = w1s.rearrange("(k p) c -> p k c", p=128)
                        w3r = w3s.rearrange("(k p) c -> p k c", p=128)
                        w2r = w2s.rearrange("(f p) d -> p f d", p=128)
                        for (g0, gn) in groups:
                            ws = gi % 2
                            gi += 1
                            sy.dma("pool", W1[ws][:, :, 0:gn * 128], w1r[:, :, g0 * 128:(g0 + gn) * 128],
                                   writes=[B_W[ws]])
                            sy.dma("pool", W3[ws][:, :, 0:gn * 128], w3r[:, :, g0 * 128:(g0 + gn) * 128],
                                   writes=[B_W[ws]])
                            sy.dma("pool", W2[ws][:, 0:gn, :], w2r[:, g0:g0 + gn, :], writes=[B_W[ws]])
                            for tbi, (o, n) in enumerate(tbs):
                                gs = tbi % 2
                                for f in range(gn):
                                    pa = (2 * f) % 4
                                    pb = (2 * f + 1) % 4
                                    for (pp, Wt) in ((pa, W1), (pb, W3)):
                                        for k in range(8):
                                            sy.op("pe", lambda k=k, pp=pp, Wt=Wt, f=f: PE.matmul(
                                                PS[pp][:, 0:n], Wt[ws][:, k, f * 128:(f + 1) * 128],
                                                h2[:, k, o:o + n], start=(k == 0), stop=(k == 7)),
                                                reads=[B_W[ws], B_h2], writes=[PSB[pp]], mark=(k == 7))
                                    s2 = f % 2
                                    sy.op("act", lambda pa=pa, s2=s2: A_.activation(
                                        out=sa[s2][:, 0:n], in_=PS[pa][:, 0:n], func=AF.Silu),
                                        reads=[PSB[pa]], writes=[B_sa[s2]])
                                    sy.op("dve", lambda pb=pb, s2=s2, f=f, gs=gs: V.tensor_tensor(
                                        out=gg[gs][:, f, 0:n], in0=PS[pb][:, 0:n], in1=sa[s2][:, 0:n], op=ALU.mult),
                                        reads=[PSB[pb], B_sa[s2]], writes=[B_gg[gs][f]])
                                for ts in range(n // 128):
                                    tl = (o // 128) + ts
                                    for dh in range(2):
                                        py = 4 + (2 * ts + dh) % 4
                                        for f in range(gn):
                                            sy.op("pe", lambda f=f, py=py, ts=ts, dh=dh, gs=gs: PE.matmul(
                                                PS[py][:], gg[gs][:, f, ts * 128:(ts + 1) * 128],
                                                W2[ws][:, f, dh * 512:(dh + 1) * 512], start=(f == 0),
                                                stop=(f == gn - 1)), reads=[B_gg[gs][f], B_W[ws]],
                                                writes=[PSB[py]], mark=(f == gn - 1))
                                        ya = yacc[:, tl, dh * 512:(dh + 1) * 512]
                                        if moe:
                                            cj = t0 - 2 + tl
                                            if first:
                                                sy.op("dve", lambda py=py, ya=ya, cj=cj, e=e: V.tensor_scalar(
                                                    out=ya, in0=PS[py][:], scalar1=comb[:, cj, e:e + 1], scalar2=None,
                                                    op0=ALU.mult), reads=[PSB[py], B_comb[cj]], writes=[B_y[tl]])
                                            else:
                                                sy.op("dve", lambda py=py, ya=ya, cj=cj, e=e: V.scalar_tensor_tensor(
                                                    out=ya, in0=PS[py][:], scalar=comb[:, cj, e:e + 1], in1=ya,
                                                    op0=ALU.mult, op1=ALU.add), reads=[PSB[py], B_comb[cj], B_y[tl]],
                                                    writes=[B_y[tl]])
                                        else:
                                            if first:
                                                sy.op("dve", lambda py=py, ya=ya: V.tensor_copy(out=ya, in_=PS[py][:]),
                                                      reads=[PSB[py]], writes=[B_y[tl]])
                                            else:
                                                sy.op("dve", lambda py=py, ya=ya: V.tensor_tensor(
                                                    out=ya, in0=PS[py][:], in1=ya, op=ALU.add),
                                                    reads=[PSB[py], B_y[tl]], writes=[B_y[tl]])
                            first = False
                    if si + 1 < len(supers):
                        load_h2(si + 1, "act")
                    def ep_load(tl):
                        t = t0 + tl
                        sy.dma("sp", xt[tl % 2][:], Xsrc[t * 128:(t + 1) * 128, :], reads=[db(Xsrc.name, t)],
                               writes=[B_xt[tl % 2]])

                    ep_load(0)
                    for tl in range(ntl):
                        t = t0 + tl
                        ty = 1 if t < 2 else 0
                        s2 = tl % 2
                        if tl + 1 < ntl:
                            ep_load(tl + 1)
                        sy.op("dve", lambda tl=tl, ty=ty: V.tensor_tensor(out=yacc[:, tl, :], in0=yacc[:, tl, :],
                                                                          in1=GTt[:, ty, :], op=ALU.mult),
                              reads=[B_y[tl], B_GT], writes=[B_y[tl]])
                        sy.op("pool", lambda tl=tl, s2=s2: G_.tensor_tensor(out=xt[s2][:], in0=xt[s2][:],
                                                                            in1=yacc[:, tl, :], op=ALU.add),
                              reads=[B_xt[s2], B_y[tl]], writes=[B_xt[s2]])
                        if not final:
                            sy.dma("sp", Xdst[t * 128:(t + 1) * 128, :], xt[s2][:], reads=[B_xt[s2]],
                                   writes=[db(Xdst.name, t)])
                        else:
                            sy.op("act", lambda s2=s2: A_.activation(out=junk[:], in_=xt[s2][:], func=AF.Square,
                                                                     accum_out=stat[s2][:, 0:1]),
                                  reads=[B_xt[s2]], writes=[B_junk, B_stat[s2]])
                            sy.op("act", lambda s2=s2: A_.activation(out=stat[s2][:, 1:2], in_=stat[s2][:, 0:1],
                                                                     func=AF.Sqrt, bias=epsT[:, 0:1], scale=1.0 / D),
                                  reads=[B_stat[s2], B_const], writes=[B_stat[s2]])
                            sy.op("dve", lambda s2=s2: V.reciprocal(out=stat[s2][:, 2:3], in_=stat[s2][:, 1:2]),
                                  reads=[B_stat[s2]], writes=[B_stat[s2]])
                            sy.op("dve", lambda s2=s2: V.scalar_tensor_tensor(
                                out=xt[s2][:], in0=xt[s2][:], scalar=stat[s2][:, 2:3], in1=fn[:], op0=ALU.mult,
                                op1=ALU.mult), reads=[B_xt[s2], B_stat[s2], B_fn], writes=[B_xt[s2]])
                            lt = t - 2
                            sy.dma("sp", out_d[lt * 128:(lt + 1) * 128, :], xt[s2][:], reads=[B_xt[s2]],
                                   writes=[db("out", lt)])
                sy.barrier()

        phases = []
        Xin = I["xin"]
        phases.append(("mod0", lambda: phase_mod(0)))
        phases.append(("n1_0", lambda: phase_norm(0, 0, Xin, 0)))
        for mx in "ABC":
            phases.append(("mix%s0" % mx, lambda mx=mx: phase_mixer(0, mx, True)))
        phases.append(("wo0", lambda: phase_wout(0, Xin, S["X1"], 0)))
        phases.append(("n2_0", lambda: phase_norm(0, 1, S["X1"], 0)))
        phases.append(("ff0", lambda: phase_ffn(0, S["X1"], S["X2"], 0, False, False)))
        phases.append(("mod1", lambda: phase_mod(1)))
        phases.append(("n1_1", lambda: phase_norm(1, 0, S["X2"], 0)))
        for mx in "ABC":
            phases.append(("mix%s1" % mx, lambda mx=mx: phase_mixer(1, mx, False)))
        phases.append(("wo1", lambda: phase_wout(1, S["X2"], S["X3"], 2)))
        phases.append(("n2_1", lambda: phase_norm(1, 1, S["X3"], 2, with_router=True)))
        phases.append(("ff1", lambda: phase_ffn(1, S["X3"], S["X4"], 2, True, True)))
        for name, fn_ in phases:
            if name in skip:
                continue
            fn_()
            PHASE_MARKS.append((name, sy.cnt["pe"]))
            if stop_after == name:
                break
        sy.barrier()
    return nc


def prep_inputs(inputs, b):
    f = lambda a: np.ascontiguousarray(np.asarray(a, dtype=np.float32))
    m = {}
    m["xin"] = f(np.concatenate([inputs["ctx"][b], inputs["x"][b]], axis=0))
    cv = np.zeros((128, 16), np.float32)
    cv[:, 0:8] = np.asarray(inputs["c"][b]).reshape(8, 128).T
    cv[:, 8:16] = np.asarray(inputs["c_ctx"]).reshape(8, 128).T
    m["cvec"] = cv
    return m


def prep_shared(inputs):
    f = lambda a: np.ascontiguousarray(np.asarray(a, dtype=np.float32))
    m = {}
    for k in ("w_mod", "b_mod", "norm_mix", "norm_ffn", "c_sinks", "w_out", "w1_dense", "w3_dense",
              "w2_dense", "w_router", "w1_moe", "w3_moe", "w2_moe", "final_norm"):
        m[k] = f(inputs[k])
    w_in = np.asarray(inputs["w_in"], dtype=np.float32)
    pa = w_in[:, :, 0:640]
    pb = w_in[:, :, 640:1056]
    pc = w_in[:, :, 1056:1696]

    def gqa_cols(p):
        q = p[:, :, 0:384]
        k0 = p[:, :, 384:448]
        k1 = p[:, :, 448:512]
        v = p[:, :, 512:640]
        return np.concatenate([q, k0, k1, k1, k0, v], axis=2)

    m["wA"] = f(gqa_cols(pa))
    m["wB"] = f(pb)
    m["wC"] = f(gqa_cols(pc))
    pv = np.zeros((128, 16), np.float32)
    aq = np.asarray(inputs["a_q_norm"], np.float32)
    ak = np.asarray(inputs["a_k_norm"], np.float32)
    bqn = np.asarray(inputs["b_q_norm"], np.float32)
    bkv = np.asarray(inputs["b_kv_norm"], np.float32)
    for li in range(DEPTH):
        pv[:, li * 5 + 0] = np.concatenate([aq[li], aq[li]])
        pv[:, li * 5 + 1] = np.concatenate([ak[li], ak[li]])
        pv[:, li * 5 + 2] = bqn[li, 0:128]
        pv[:, li * 5 + 3] = bqn[li, 128:256]
        pv[:, li * 5 + 4] = bkv[li]
    m["pvec"] = pv
    wukv = np.asarray(inputs["w_ukv"], np.float32).reshape(DEPTH, 128, 4, 128)
    wk = np.zeros((DEPTH, 128, 4, 128), np.float32)
    wk[:, :, :, 0:64] = wukv[:, :, :, 0:64]
    m["wukv_k"] = f(wk.reshape(DEPTH, 128, 512))
    wq = np.zeros((DEPTH, 256, 4, 128), np.float32)
    wq[:, :, :, 0:96] = np.asarray(inputs["w_uq"], np.float32).reshape(DEPTH, 256, 4, 96)
    m["w_uq"] = f(wq.reshape(DEPTH, 256, 512))
    m["wukv_v"] = f(wukv[:, :, :, 64:128].reshape(DEPTH, 128, 256))
    m.update(make_consts())
    return m


_CACHE = {}


def kernel(**inputs):
    inputs = {k: np.asarray(v) for k, v in inputs.items()}
    if "nc" not in _CACHE:
        _CACHE["nc"] = build()
    nc = _CACHE["nc"]
    shared = prep_shared(inputs)
    in_maps = []
    for b in range(8):
        m = dict(shared)
        m.update(prep_inputs(inputs, b))
        in_maps.append(m)
    res = run_bass_kernel_spmd(nc, in_maps, core_ids=list(range(8)))
    out = np.stack([np.asarray(r["out"], dtype=np.float32) for r in res.results], axis=0)
    return out
```

```python
import contextlib
import numpy as np
import ml_dtypes
import concourse.bass as bass
import concourse.mybir as mybir
from concourse.bass_utils import run_bass_kernel_spmd

F32 = mybir.dt.float32
BF16 = mybir.dt.bfloat16
AF = mybir.ActivationFunctionType
ALU = mybir.AluOpType
AX = mybir.AxisListType

D = 1024
NCTX = 256
SEQ = 4096
T = NCTX + SEQ
NT = T // 128
DEPTH = 2
HD = 64
EPS = 1e-6
DFF_D = 2816
DFF_E = 3584
NE = 8
BLOCKS = [(0, 256)] + [(256 + 512 * i, 512) for i in range(8)]


class Buf:
    __slots__ = ("name", "w", "r", "dsem")

    def __init__(self, name, dsem=None):
        self.name = name
        self.w = None
        self.r = {}
        self.dsem = dsem


class Sync:
    ENG = ("pe", "act", "dve", "pool", "sp")

    def __init__(self, nc, es, n_dma_sems=56):
        self.nc = nc
        self.eng = {"pe": nc.tensor, "act": nc.scalar, "dve": nc.vector, "pool": nc.gpsimd, "sp": nc.sync}
        self.semobj = {}
        self.cnt = {}
        for e in ("pe", "act", "dve", "pool"):
            self.semobj[e] = es.enter_context(nc.semaphore("s_" + e))
            self.cnt[e] = 0
        self.dma_keys = []
        for i in range(n_dma_sems):
            k = "d%d" % i
            self.semobj[k] = es.enter_context(nc.semaphore("s_" + k))
            self.cnt[k] = 0
            self.dma_keys.append(k)
        self.next_dma = 0
        self.next_sw = 0
        self.n_sw = 8
        self.known = {e: {} for e in self.ENG}
        self.pend_r = []
        self.pend_w = []

    def new_dsem(self, sw=False):
        if sw:
            k = self.dma_keys[self.next_sw % self.n_sw]
            self.next_sw += 1
            return k
        n = len(self.dma_keys) - self.n_sw
        k = self.dma_keys[self.n_sw + self.next_dma % n]
        self.next_dma += 1
        return k

    def buf(self, name, dma=False, sw=False):
        return Buf(name, self.new_dsem(sw) if (dma or sw) else None)

    def _wait(self, e, key, val):
        if self.known[e].get(key, 0) >= val:
            return
        self.eng[e].wait_ge(self.semobj[key], val)
        self.known[e][key] = val

    def _deps(self, e, reads, writes):
        need = {}
        for b in reads:
            if b.w is not None:
                k, v = b.w
                need[k] = max(need.get(k, 0), v)
        for b in writes:
            if b.w is not None:
                k, v = b.w
                need[k] = max(need.get(k, 0), v)
            for k, v in b.r.items():
                if k != e:
                    need[k] = max(need.get(k, 0), v)
        for k, v in need.items():
            if e == "pe" and k == "pe":
                continue
            self._wait(e, k, v)

    def op(self, e, fn, reads=(), writes=(), mark=True):
        for b in list(reads) + list(writes):
            if e != "pe":
                assert not (b in self.pend_w), "pending PE write on %s" % b.name
        for b in writes:
            if e != "pe":
                assert not (b in self.pend_r), "pending PE read on %s" % b.name
        self._deps(e, reads, writes)
        ins = fn()
        if e == "pe":
            for b in reads:
                if b not in self.pend_r:
                    self.pend_r.append(b)
            for b in writes:
                if b not in self.pend_w:
                    self.pend_w.append(b)
            if mark:
                self.cnt["pe"] += 1
                ins.then_inc(self.semobj["pe"], 1)
                c = self.cnt["pe"]
                for b in self.pend_w:
                    b.w = ("pe", c)
                    b.r = {}
                for b in self.pend_r:
                    if b not in self.pend_w:
                        b.r["pe"] = c
                self.pend_r = []
                self.pend_w = []
            return ins
        self.cnt[e] += 1
        ins.then_inc(self.semobj[e], 1)
        ev = (e, self.cnt[e])
        for b in reads:
            b.r[e] = ev[1]
        for b in writes:
            b.w = ev
            b.r = {}
        return ins

    def dma(self, q, out, in_, reads=(), writes=(), key=None):
        if key is None:
            for b in list(writes) + list(reads):
                if b.dsem is not None:
                    key = b.dsem
                    break
        assert key is not None
        for b in list(reads) + list(writes):
            assert b not in self.pend_w and not (b in writes and b in self.pend_r), "pending PE on %s" % b.name
        self._deps(q, reads, writes)
        assert (q == "pool") == (self.dma_keys.index(key) < self.n_sw), "dma sem pool mismatch"
        ins = self.eng[q].dma_start(out=out, in_=in_)
        self.cnt[key] += 16
        ins.then_inc(self.semobj[key], 16)
        ev = (key, self.cnt[key])
        for b in reads:
            b.r[key] = ev[1]
        for b in writes:
            b.w = ev
            b.r = {}
        return ins

    def barrier(self):
        assert not self.pend_r and not self.pend_w
        for e in self.ENG:
            for k, v in self.cnt.items():
                if v > 0 and not (e == "pe" and k == "pe"):
                    self._wait(e, k, v)


def _rope_tables(rot_dim):
    rows = SEQ // 64
    row, col = np.meshgrid(np.arange(rows), np.arange(64), indexing="ij")
    row = row.reshape(-1).astype(np.float32)
    col = col.reshape(-1).astype(np.float32)
    n_freq = rot_dim // 4
    inv_freq = (np.float32(10000.0) ** (-np.arange(n_freq, dtype=np.float32) / np.float32(n_freq))).astype(np.float32)
    ang_r = row[:, None] * inv_freq
    ang_c = col[:, None] * inv_freq
    ang = np.concatenate([ang_r, ang_r, ang_c, ang_c], axis=-1).astype(np.float32)
    return np.cos(ang).astype(np.float32), np.sin(ang).astype(np.float32)


def _rot_perm(rot_dim):
    nf = rot_dim // 4
    P = np.zeros((rot_dim, rot_dim), np.float32)
    for a in range(2):
        b0 = a * 2 * nf
        for f in range(nf):
            P[b0 + nf + f, b0 + f] = -1.0
            P[b0 + f, b0 + nf + f] = 1.0
    return P


def make_consts():
    c = {}
    cosH, sinH = _rope_tables(64)
    cosT = np.ones((128, T), np.float32)
    sinT = np.zeros((128, T), np.float32)
    cosT[0:64, NCTX:] = cosH.T
    cosT[64:128, NCTX:] = cosH.T
    sinT[0:64, NCTX:] = sinH.T
    sinT[64:128, NCTX:] = sinH.T
    c["cosH"] = cosT
    c["sinH"] = sinT
    cosR, sinR = _rope_tables(32)
    cR = np.ones((128, T), np.float32)
    sR = np.zeros((128, T), np.float32)
    cR[64:96, NCTX:] = cosR.T
    sR[64:96, NCTX:] = sinR.T
    c["cosR"] = cR
    c["sinR"] = sR
    cK = np.ones((32, T), np.float32)
    sK = np.zeros((32, T), np.float32)
    cK[:, NCTX:] = cosR.T
    sK[:, NCTX:] = sinR.T
    c["cosK"] = cK
    c["sinK"] = sK
    P64 = _rot_perm(64)
    Pm128 = np.zeros((128, 128), np.float32)
    Pm128[0:64, 0:64] = P64
    Pm128[64:128, 64:128] = P64
    P32 = _rot_perm(32)
    Pm96 = np.zeros((96, 96), np.float32)
    Pm96[64:96, 64:96] = P32
    bo = np.zeros((128, 128), np.float32)
    bo[0:64, 0:64] = 1.0
    bo[64:128, 64:128] = 1.0
    sel = np.zeros((32, 128), np.float32)
    sel[np.arange(32), 64 + np.arange(32)] = 1.0
    cm = np.zeros((128, 7, 128), np.float32)
    cm[:, 0, :] = Pm128
    cm[0:96, 1, 0:96] = Pm96
    cm[:, 2, :] = bo
    cm[:, 3, :] = 1.0
    cm[:, 4, :] = np.eye(128, dtype=np.float32)
    cm[0:32, 5, 0:32] = P32
    cm[0:32, 6, :] = sel
    c["cmat"] = cm.astype(ml_dtypes.bfloat16)
    c["identf"] = np.eye(128, dtype=np.float32)
    i = np.arange(128)[:, None]
    j = np.arange(512)[None, :]
    m = np.zeros((128, 6, 512), np.float32)
    for ci, cc in enumerate(range(-1, 5)):
        rel = cc * 128 + i - j
        m[:, ci, :] = (np.abs(rel) <= 128).astype(np.float32)
    c["wmask"] = m.astype(ml_dtypes.bfloat16)
    return c


CONST_SHAPES = {
    "cosH": ([128, T], F32), "sinH": ([128, T], F32), "cosR": ([128, T], F32), "sinR": ([128, T], F32),
    "cosK": ([32, T], F32), "sinK": ([32, T], F32), "cmat": ([128, 7, 128], BF16), "identf": ([128, 128], F32),
    "wmask": ([128, 6, 512], BF16),
}

IN_SHAPES = {
    "xin": [T, D], "cvec": [128, 16], "w_mod": [DEPTH, D, 6 * D], "b_mod": [DEPTH, 6 * D],
    "norm_mix": [DEPTH, D], "norm_ffn": [DEPTH, D], "wA": [DEPTH, D, 768], "wB": [DEPTH, D, 416],
    "wC": [DEPTH, D, 768], "pvec": [128, 16], "w_uq": [DEPTH, 256, 512], "wukv_k": [DEPTH, 128, 512],
    "wukv_v": [DEPTH, 128, 256], "c_sinks": [DEPTH, 6], "w_out": [DEPTH, D, D],
    "w1_dense": [1, D, DFF_D], "w3_dense": [1, D, DFF_D], "w2_dense": [1, DFF_D, D],
    "w_router": [1, D, NE], "w1_moe": [1, NE, D, DFF_E], "w3_moe": [1, NE, D, DFF_E], "w2_moe": [1, NE, DFF_E, D],
    "final_norm": [D],
}


PHASE_MARKS = []


def build(debug=False, stop_after=None, skip=()):
    del PHASE_MARKS[:]
    nc = bass.Bass("TRN2", target_bir_lowering=False)
    I = {}
    for k, shp in IN_SHAPES.items():
        I[k] = nc.dram_tensor(k, shp, F32, kind="ExternalInput").ap()
    for k, (shp, dt) in CONST_SHAPES.items():
        I[k] = nc.dram_tensor(k, shp, dt, kind="ExternalInput").ap()
    out_d = nc.dram_tensor("out", [SEQ, D], F32, kind="ExternalOutput").ap()
    skind = "ExternalOutput" if debug else "Internal"
    S = {}

    def scratch(name, shp, dt):
        S[name] = nc.dram_tensor(name, shp, dt, kind=skind).ap()

    scratch("modsc", [2, 128, 6 * D], F32)
    scratch("hT", [8, 128, T], BF16)
    scratch("mixT", [8, 128, T], BF16)
    scratch("X1", [T, D], F32)
    scratch("X2", [T, D], F32)
    scratch("X3", [T, D], F32)
    scratch("X4", [T, D], F32)

    es = contextlib.ExitStack()
    with es:
        sy = Sync(nc, es)
        V, G_, Pq = nc.vector, nc.gpsimd, nc.sync
        A_, PE = nc.scalar, nc.tensor

        uid = [0]

        def sb(st, name, shp, dt):
            uid[0] += 1
            return st.enter_context(nc.sbuf_tensor("sb%d_%s" % (uid[0], name), shp, dt))

        PSALL = es.enter_context(nc.psum_tensor("psall", [128, 8 * 512], F32))
        PS = [PSALL[:, i * 512:(i + 1) * 512] for i in range(8)]
        PSB = [sy.buf("ps%d" % i) for i in range(8)]

        cmat = sb(es, "cmat", [128, 7, 128], BF16)
        identf = sb(es, "identf", [128, 128], F32)
        epsT = sb(es, "epsT", [128, 1], F32)
        pvec = sb(es, "pvec", [128, 16], F32)
        comb = sb(es, "comb", [128, 32, 8], F32)
        B_const = sy.buf("const", dma=True)
        B_comb = [sy.buf("comb%d" % i) for i in range(32)]
        sy.dma("sp", cmat[:], I["cmat"], writes=[B_const])
        sy.dma("sp", identf[:], I["identf"], writes=[B_const])
        sy.dma("sp", pvec[:], I["pvec"], writes=[B_const])
        sy.op("pool", lambda: G_.memset(epsT[:], EPS), writes=[B_const])
        PM128, PM96, BONES, AONES, IDENT = (cmat[:, 0, :], cmat[:, 1, :], cmat[:, 2, :], cmat[:, 3, :],
                                            cmat[:, 4, :])
        PM32 = cmat[0:32, 5, 0:32]
        SEL32 = cmat[0:32, 6, :]

        DB = {}

        def db(name, idx):
            k = (name, idx)
            if k not in DB:
                DB[k] = Buf("%s_%s" % (name, idx))
            return DB[k]

        state = {"rr": 0}

        def rr(lst):
            state["rr"] += 1
            return lst[state["rr"] % len(lst)]

        def phase_mod(li):
            with contextlib.ExitStack() as st:
                cc = sb(st, "cc", [128, 16], F32)
                sil = sb(st, "sil", [128, 16], BF16)
                cl = sb(st, "cl", [128, 16, 128], BF16)
                bmod = sb(st, "bmod", [128, 6 * D], F32)
                nmx = sb(st, "nmx", [128, 2, D], F32)
                modt = [sb(st, "modt%d" % i, [128, 6 * D], F32) for i in range(2)]
                wm = [sb(st, "wm%d" % i, [128, 8, 512], BF16) for i in range(2)]
                B_cc, B_sil, B_cl = sy.buf("cc", dma=True), sy.buf("sil"), sy.buf("cl")
                B_bmod, B_nmx = sy.buf("bmod", dma=True), sy.buf("nmx", dma=True)
                B_modt = [[sy.buf("modt%d_%d" % (i, j)) for j in range(12)] for i in range(2)]
                B_modst = [sy.buf("modst%d" % i, dma=True) for i in range(2)]
                B_wm = [sy.buf("wm%d" % i, sw=True) for i in range(2)]
                sy.dma("sp", cc[:], I["cvec"], writes=[B_cc])
                sy.dma("sp", bmod[:], I["b_mod"][li].partition_broadcast(128), writes=[B_bmod])
                sy.dma("sp", nmx[:, 0, :], I["norm_mix"][li].partition_broadcast(128), writes=[B_nmx])
                sy.dma("sp", nmx[:, 1, :], I["norm_ffn"][li].partition_broadcast(128), writes=[B_nmx])
                sy.op("act", lambda: A_.activation(out=sil[:], in_=cc[:], func=AF.Silu), reads=[B_cc], writes=[B_sil])
                sy.op("dve", lambda: V.tensor_copy(out=cl[:], in_=sil[:].unsqueeze(2).to_broadcast([128, 16, 128])),
                      reads=[B_sil], writes=[B_cl])
                wsrc = I["w_mod"][li].rearrange("(k p) c -> p k c", p=128)
                for j in range(12):
                    s = j % 2
                    sy.dma("pool", wm[s][:], wsrc[:, :, j * 512:(j + 1) * 512], writes=[B_wm[s]])
                    for ty in range(2):
                        pi = (2 * j + ty) % 4
                        for k in range(8):
                            sy.op("pe", lambda k=k, ty=ty, pi=pi, s=s: PE.matmul(
                                PS[pi][:], cl[:, ty * 8 + k, :], wm[s][:, k, :], start=(k == 0), stop=(k == 7)),
                                reads=[B_cl, B_wm[s]], writes=[PSB[pi]], mark=(k == 7))
                        sy.op("dve", lambda ty=ty, pi=pi, j=j: V.tensor_tensor(
                            out=modt[ty][:, j * 512:(j + 1) * 512], in0=PS[pi][:], in1=bmod[:, j * 512:(j + 1) * 512],
                            op=ALU.add), reads=[PSB[pi], B_bmod], writes=[B_modt[ty][j]])
                for ty in range(2):
                    for (c0, ni) in ((1024, 0), (4096, 1)):
                        bl = [B_modt[ty][c0 // 512], B_modt[ty][c0 // 512 + 1]]
                        sy.op("dve", lambda ty=ty, c0=c0, ni=ni: V.scalar_tensor_tensor(
                            out=modt[ty][:, c0:c0 + 1024], in0=modt[ty][:, c0:c0 + 1024], scalar=1.0,
                            in1=nmx[:, ni, :], op0=ALU.add, op1=ALU.mult), reads=bl + [B_nmx], writes=bl)
                    sy.dma("sp", S["modsc"][ty], modt[ty][:], reads=B_modt[ty] + [B_modst[ty]],
                           writes=[db("modsc", ty)], key=B_modst[ty].dsem)
                sy.barrier()

        def load_mod(st, names):
            res = {}
            for tag, c0 in names:
                t = sb(st, "md_" + tag, [128, 2, D], F32)
                b = sy.buf("md_" + tag, dma=True)
                for ty in range(2):
                    sy.dma("sp", t[:, ty, :], S["modsc"][ty][:, c0:c0 + D], reads=[db("modsc", ty)], writes=[b])
                res[tag] = (t, b)
            return res

        def phase_norm(li, which, Xsrc, ntiles_from, with_router=False):
            with contextlib.ExitStack() as st:
                md = load_mod(st, [("G", 1024 if which == 0 else 4096), ("S", 0 if which == 0 else 3072)])
                (Gt, B_G), (St, B_S) = md["G"], md["S"]
                xt = [sb(st, "xt%d" % i, [128, D], F32) for i in range(3)]
                B_xt = [sy.buf("xt%d" % i, dma=True) for i in range(3)]
                junk = sb(st, "junk", [128, D], BF16)
                B_junk = sy.buf("junk")
                stat = [sb(st, "stat%d" % i, [128, 4], F32) for i in range(3)]
                B_stat = [sy.buf("stat%d" % i) for i in range(3)]
                tmp = [sb(st, "tmp%d" % i, [128, D], F32) for i in range(2)]
                B_tmp = [sy.buf("tmp%d" % i) for i in range(2)]
                hb = [sb(st, "hb%d" % i, [128, D], BF16) for i in range(2)]
                B_hb = [sy.buf("hb%d" % i) for i in range(2)]
                hTb = [sb(st, "hTb%d" % i, [128, 8, 512], BF16) for i in range(2)]
                B_hTb = [sy.buf("hTb%d" % i, dma=True) for i in range(2)]
                if with_router:
                    wr = sb(st, "wr", [128, 8, NE], F32)
                    B_wr = sy.buf("wr", dma=True)
                    sy.dma("sp", wr[:], I["w_router"][0].rearrange("(k p) e -> p k e", p=128), writes=[B_wr])
                    hfT = [sb(st, "hfT%d" % i, [128, 8, 128], F32) for i in range(2)]
                    B_hfT = [sy.buf("hfT%d" % i) for i in range(2)]
                    lg = [sb(st, "lg%d" % i, [128, 4, NE], F32) for i in range(2)]
                    B_lg = [sy.buf("lg%d" % i) for i in range(2)]
                    sm = [sb(st, "sm%d" % i, [128, 8], F32) for i in range(2)]
                    B_sm = [sy.buf("sm%d" % i) for i in range(2)]
                hTd = S["hT"].rearrange("k p t -> p k t")
                jobs = []
                for (c0, n) in BLOCKS:
                    tiles = [c0 // 128 + i for i in range(n // 128)]
                    tiles = [t for t in tiles if t >= ntiles_from]
                    if not tiles:
                        continue
                    hs = rr([0, 1])
                    for ti, t in enumerate(tiles):
                        jobs.append((t, ti, hs, tiles, ti == len(tiles) - 1))

                def stageL(job):
                    t, ti, hs, tiles, lastb = job
                    ty = 1 if t < 2 else 0
                    s3 = t % 3
                    s2 = t % 2
                    sy.dma("sp", xt[s3][:], Xsrc[t * 128:(t + 1) * 128, :], reads=[db(Xsrc.name, t)],
                           writes=[B_xt[s3]])

                def stageA(job):
                    t, ti, hs, tiles, lastb = job
                    ty = 1 if t < 2 else 0
                    s3 = t % 3
                    s2 = t % 2
                    sy.op("act", lambda s3=s3: A_.activation(out=junk[:], in_=xt[s3][:], func=AF.Square,
                                                             accum_out=stat[s3][:, 0:1]),
                          reads=[B_xt[s3]], writes=[B_junk, B_stat[s3]])
                    sy.op("act", lambda s3=s3: A_.activation(out=stat[s3][:, 1:2], in_=stat[s3][:, 0:1],
                                                             func=AF.Sqrt, bias=epsT[:, 0:1], scale=1.0 / D),
                          reads=[B_stat[s3], B_const], writes=[B_stat[s3]])
                    sy.op("dve", lambda s3=s3: V.reciprocal(out=stat[s3][:, 2:3], in_=stat[s3][:, 1:2]),
                          reads=[B_stat[s3]], writes=[B_stat[s3]])
                    sy.op("dve", lambda s3=s3, s2=s2, ty=ty: V.scalar_tensor_tensor(
                        out=tmp[s2][:], in0=xt[s3][:], scalar=stat[s3][:, 2:3], in1=Gt[:, ty, :],
                        op0=ALU.mult, op1=ALU.mult), reads=[B_xt[s3], B_stat[s3], B_G], writes=[B_tmp[s2]])
                    if with_router:
                        sy.op("pool", lambda s2=s2, ty=ty: G_.tensor_tensor(
                            out=tmp[s2][:], in0=tmp[s2][:], in1=St[:, ty, :], op=ALU.add),
                            reads=[B_tmp[s2], B_S], writes=[B_tmp[s2]])
                        sy.op("act", lambda s2=s2: A_.copy(out=hb[s2][:], in_=tmp[s2][:]),
                              reads=[B_tmp[s2]], writes=[B_hb[s2]])
                    else:
                        sy.op("pool", lambda s2=s2, ty=ty: G_.tensor_tensor(
                            out=hb[s2][:], in0=tmp[s2][:], in1=St[:, ty, :], op=ALU.add),
                            reads=[B_tmp[s2], B_S], writes=[B_hb[s2]])

                def stageB(job):
                    t, ti, hs, tiles, lastb = job
                    ty = 1 if t < 2 else 0
                    s3 = t % 3
                    s2 = t % 2
                    pi = t % 2
                    pst = PS[pi][:].bitcast(BF16)
                    for k in range(8):
                        sy.op("pe", lambda k=k, s2=s2, pst=pst: PE.transpose(
                            pst[:, k * 128:(k + 1) * 128], hb[s2][:, k * 128:(k + 1) * 128], IDENT),
                            reads=[B_hb[s2], B_const], writes=[PSB[pi]], mark=(k == 7))
                    sy.op("act", lambda ti=ti, hs=hs, pst=pst: A_.copy(
                        out=hTb[hs][:, :, ti * 128:(ti + 1) * 128],
                        in_=pst.rearrange("p (k t) -> p k t", k=8)),
                        reads=[PSB[pi]], writes=[B_hTb[hs]])
                    if with_router:
                        j = t - 2
                        for half in range(2):
                            pj = 2 + half
                            for k4 in range(4):
                                k = half * 4 + k4
                                sy.op("pe", lambda k=k, k4=k4, s2=s2, pj=pj: PE.transpose(
                                    PS[pj][:, k4 * 128:(k4 + 1) * 128], tmp[s2][:, k * 128:(k + 1) * 128],
                                    identf[:]), reads=[B_tmp[s2], B_const], writes=[PSB[pj]], mark=(k4 == 3))
                            sy.op("dve", lambda half=half, s2=s2, pj=pj: V.tensor_copy(
                                out=hfT[s2][:, half * 4:(half + 1) * 4, :],
                                in_=PS[pj][:].rearrange("p (k t) -> p k t", k=4)),
                                reads=[PSB[pj]], writes=[B_hfT[s2]])
                        for k in range(8):
                            sy.op("pe", lambda k=k, s2=s2: PE.matmul(
                                PS[4][:, 0:NE], hfT[s2][:, k, :], wr[:, k, :], start=(k == 0), stop=(k == 7)),
                                reads=[B_hfT[s2], B_wr], writes=[PSB[4]], mark=(k == 7))
                        L = lg[s2]
                        smt = sm[s2]
                        bl = [B_lg[s2], B_sm[s2]]
                        sy.op("dve", lambda L=L: V.tensor_copy(out=L[:, 0, :], in_=PS[4][:, 0:NE]),
                              reads=[PSB[4]], writes=bl)
                        sy.op("dve", lambda L=L, smt=smt: V.tensor_reduce(
                            out=smt[:, 0:1], in_=L[:, 0, :], axis=AX.X, op=ALU.max), reads=bl, writes=bl)
                        sy.op("dve", lambda L=L, smt=smt: V.tensor_scalar(
                            out=L[:, 1, :], in0=L[:, 0, :], scalar1=smt[:, 0:1], scalar2=None,
                            op0=ALU.is_equal), reads=bl, writes=bl)
                        sy.op("dve", lambda L=L: V.scalar_tensor_tensor(
                            out=L[:, 2, :], in0=L[:, 1, :], scalar=-1e30, in1=L[:, 0, :],
                            op0=ALU.mult, op1=ALU.add), reads=bl, writes=bl)
                        sy.op("dve", lambda L=L, smt=smt: V.tensor_reduce(
                            out=smt[:, 1:2], in_=L[:, 2, :], axis=AX.X, op=ALU.max), reads=bl, writes=bl)
                        sy.op("dve", lambda L=L, smt=smt: V.tensor_scalar(
                            out=L[:, 3, :], in0=L[:, 2, :], scalar1=smt[:, 1:2], scalar2=None,
                            op0=ALU.is_equal), reads=bl, writes=bl)
                        sy.op("dve", lambda smt=smt: V.tensor_tensor(
                            out=smt[:, 2:3], in0=smt[:, 1:2], in1=smt[:, 0:1], op=ALU.subtract),
                            reads=bl, writes=bl)
                        sy.op("act", lambda smt=smt: A_.activation(out=smt[:, 3:4], in_=smt[:, 2:3], func=AF.Exp),
                              reads=bl, writes=bl)
                        sy.op("dve", lambda smt=smt: V.tensor_scalar(
                            out=smt[:, 4:5], in0=smt[:, 3:4], scalar1=1.0, scalar2=None, op0=ALU.add),
                            reads=bl, writes=bl)
                        sy.op("dve", lambda smt=smt: V.reciprocal(out=smt[:, 5:6], in_=smt[:, 4:5]),
                              reads=bl, writes=bl)
                        sy.op("dve", lambda smt=smt: V.tensor_tensor(
                            out=smt[:, 6:7], in0=smt[:, 3:4], in1=smt[:, 5:6], op=ALU.mult),
                            reads=bl, writes=bl)
                        sy.op("dve", lambda L=L, smt=smt: V.tensor_scalar(
                            out=L[:, 1, :], in0=L[:, 1, :], scalar1=smt[:, 5:6], scalar2=None, op0=ALU.mult),
                            reads=bl, writes=bl)
                        sy.op("dve", lambda L=L, smt=smt, j=j: V.scalar_tensor_tensor(
                            out=comb[:, j, :], in0=L[:, 3, :], scalar=smt[:, 6:7], in1=L[:, 1, :],
                            op0=ALU.mult, op1=ALU.add), reads=bl, writes=[B_comb[j]])

                    if lastb:
                        tb0 = tiles[0] * 128
                        nn = len(tiles) * 128
                        sy.dma("sp", hTd[:, :, tb0:tb0 + nn], hTb[hs][:, :, 0:nn], reads=[B_hTb[hs]],
                               writes=[db("hT", t_) for t_ in tiles])

                nj = len(jobs)
                for q in range(min(3, nj)):
                    stageL(jobs[q])
                stageA(jobs[0])
                for q in range(nj):
                    if q + 1 < nj:
                        stageA(jobs[q + 1])
                    stageB(jobs[q])
                    if q + 3 < nj:
                        stageL(jobs[q + 3])
                sy.barrier()

        def phase_mixer(li, mx, with_ctx):
            gl = (mx in "AC")
            with contextlib.ExitStack() as st:
                if gl:
                    wsrc = I["wA" if mx == "A" else "wC"][li]
                    wcols = 768
                    qT = sb(st, "qT", [128, 3, T], BF16)
                    kT = sb(st, "kT", [128, 2, T], BF16)
                    Va = sb(st, "Va", [128, NT, 2, 192], BF16)
                else:
                    wsrc = I["wB"][li]
                    wcols = 416
                    qT = sb(st, "qT", [128, 4, T], BF16)
                    kT = sb(st, "kT", [128, 4, T], BF16)
                    Va = sb(st, "Va", [128, NT, 4, 128], BF16)
                    wuq = sb(st, "wuq", [128, 2, 512], BF16)
                    wuk = sb(st, "wuk", [128, 512], BF16)
                    wuv = sb(st, "wuv", [128, 256], BF16)
                win = sb(st, "win", [128, 8, wcols], BF16)
                B_w = sy.buf("win", sw=True)
                sy.dma("pool", win[:], wsrc.rearrange("(k p) c -> p k c", p=128), writes=[B_w])
                if not gl:
                    sy.dma("pool", wuq[:], I["w_uq"][li].rearrange("(k p) c -> p k c", p=128), writes=[B_w])
                    sy.dma("pool", wuk[:], I["wukv_k"][li], writes=[B_w])
                    sy.dma("pool", wuv[:], I["wukv_v"][li], writes=[B_w])
                B_q = {}
                B_k = {}
                B_v = [sy.buf("v%d" % t) for t in range(NT)]
                B_vones = sy.buf("vones")

                def bq(key):
                    if key not in B_q:
                        B_q[key] = sy.buf("q%s" % (key,))
                    return B_q[key]

                def bk(key):
                    if key not in B_k:
                        B_k[key] = sy.buf("k%s" % (key,))
                    return B_k[key]

                if gl:
                    sy.op("pool", lambda: G_.memset(Va[:, :, :, 0:64], 1.0), writes=[B_vones])
                    sy.op("pool", lambda: G_.memset(Va[:, :, :, 128:192], 1.0), writes=[B_vones])
                else:
                    for h in range(4):
                        o = 64 if h % 2 == 0 else 0
                        sy.op("pool", lambda h=h, o=o: G_.memset(Va[:, :, h, o:o + 64], 1.0), writes=[B_vones])
                if mx == "C":
                    wmask = sb(st, "wmask", [128, 6, 512], BF16)
                    B_wmask = sy.buf("wmask", dma=True)
                    sy.dma("sp", wmask[:], I["wmask"], writes=[B_wmask])
                    sk = sb(st, "sk", [128, 8], F32)
                    B_sk = sy.buf("sk", dma=True)
                    sy.dma("sp", sk[:, 0:6], I["c_sinks"][li].partition_broadcast(128), writes=[B_sk])
                    sy.op("act", lambda: A_.activation(out=sk[:, 0:6], in_=sk[:, 0:6], func=AF.Exp),
                          reads=[B_sk], writes=[B_sk])

                hTb = [sb(st, "hTb%d" % i, [128, 8, 512], BF16) for i in range(2)]
                B_hTb = [sy.buf("hTb%d" % i, dma=True) for i in range(2)]
                cs = [sb(st, "cs%d" % i, [128, 2, 512], F32) for i in range(2)]
                B_cs = [sy.buf("cs%d" % i, dma=True) for i in range(2)]
                if not gl:
                    csK = [sb(st, "csK%d" % i, [32, 2, 512], F32) for i in range(2)]
                NS = 4 if gl else 3
                ub = [sb(st, "ub%d" % i, [128, 512], BF16) for i in range(NS)]
                B_ub = [sy.buf("ub%d" % i) for i in range(NS)]
                sq = [sb(st, "sq%d" % i, [128, 512], BF16) for i in range(3)]
                B_sq = [sy.buf("sq%d" % i) for i in range(3)]
                f1 = [sb(st, "f1_%d" % i, [128, 512], F32) for i in range(NS)]
                B_f1 = [sy.buf("f1_%d" % i) for i in range(NS)]
                f2 = [sb(st, "f2_%d" % i, [128, 512], F32) for i in range(NS)]
                B_f2 = [sy.buf("f2_%d" % i) for i in range(NS)]
                rs = [sb(st, "rs%d" % i, [128, 512], F32) for i in range(NS)]
                B_rs = [sy.buf("rs%d" % i) for i in range(NS)]
                if not gl:
                    cqf = [sb(st, "cqf%d" % i, [128, 512], F32) for i in range(3)]
                    B_cqf = [sy.buf("cqf%d" % i) for i in range(3)]
                    cqn = [sb(st, "cqn%d" % i, [128, 512], BF16) for i in range(3)]
                    B_cqn = [sy.buf("cqn%d" % i) for i in range(3)]
                    krf = sb(st, "krf", [32, 512], BF16)
                    B_krf = sy.buf("krf")

                hTd = S["hT"].rearrange("k p t -> p k t")
                cnt = {"s": 0}

                def nxt(n):
                    cnt["s"] += 1
                    return cnt["s"] % n

                lc = li * 5

                def rope_store(pi, n, dst_ap, dstbuf, cs_t, B_cst, rows, pm, rstd=None, gain_col=None):
                    s = rope_s1(pi, n, rows, gain_col)
                    rope_s2(s, n, dst_ap, dstbuf, cs_t, B_cst, rows, pm, rstd=rstd, gain_col=gain_col)

                def rope_s1(pi, n, rows, gain_col):
                    ubcnt[0] += 1
                    s = ubcnt[0] % NS
                    if gain_col is not None:
                        sy.op("act", lambda: A_.activation(out=ub[s][0:rows, 0:n], in_=PS[pi][0:rows, 0:n],
                                                           func=AF.Copy, scale=pvec[0:rows, gain_col:gain_col + 1]),
                              reads=[PSB[pi], B_const], writes=[B_ub[s]])
                    else:
                        sy.op("act", lambda: A_.copy(out=ub[s][0:rows, 0:n], in_=PS[pi][0:rows, 0:n]),
                              reads=[PSB[pi]], writes=[B_ub[s]])
                    return s

                def rope_s2(s, n, dst_ap, dstbuf, cs_t, B_cst, rows, pm, rstd=None, gain_col=None):
                    pr = 4 + nxt(2)
                    if callable(rstd):
                        rstd = rstd()
                    sy.op("pe", lambda: PE.matmul(PS[pr][0:rows, 0:n], pm, ub[s][0:rows, 0:n], start=True, stop=True),
                          reads=[B_ub[s], B_const], writes=[PSB[pr]])
                    if gain_col is not None:
                        sy.op("dve", lambda: V.tensor_tensor(out=f1[s][0:rows, 0:n], in0=ub[s][0:rows, 0:n],
                                                             in1=cs_t[0:rows, 0, 0:n], op=ALU.mult),
                              reads=[B_ub[s], B_cst], writes=[B_f1[s]])
                    else:
                        sy.op("dve", lambda: V.tensor_tensor(out=f1[s][0:rows, 0:n], in0=ub[s][0:rows, 0:n],
                                                             in1=cs_t[0:rows, 0, 0:n], op=ALU.mult),
                              reads=[B_ub[s], B_cst], writes=[B_f1[s]])
                    sy.op("dve", lambda: V.tensor_tensor(out=f2[s][0:rows, 0:n], in0=PS[pr][0:rows, 0:n],
                                                         in1=cs_t[0:rows, 1, 0:n], op=ALU.mult),
                          reads=[PSB[pr], B_cst], writes=[B_f2[s]])
                    if rstd is None:
                        sy.op("pool", lambda: G_.tensor_tensor(out=dst_ap, in0=f1[s][0:rows, 0:n],
                                                               in1=f2[s][0:rows, 0:n], op=ALU.add),
                              reads=[B_f1[s], B_f2[s]], writes=[dstbuf])
                    else:
                        rt, B_rt = rstd
                        sy.op("pool", lambda: G_.tensor_tensor(out=f1[s][0:rows, 0:n], in0=f1[s][0:rows, 0:n],
                                                               in1=f2[s][0:rows, 0:n], op=ALU.add),
                              reads=[B_f1[s], B_f2[s]], writes=[B_f1[s]])
                        sy.op("dve", lambda: V.tensor_tensor(out=dst_ap, in0=f1[s][0:rows, 0:n],
                                                             in1=rt[0:rows, 0:n], op=ALU.mult),
                              reads=[B_f1[s], B_rt], writes=[dstbuf])

                def rstd_from(pss, n, scale, rows=128):
                    s = nxt(NS)
                    sy.op("act", lambda: A_.activation(out=rs[s][0:rows, 0:n], in_=PS[pss][0:rows, 0:n], func=AF.Ln,
                                                       bias=epsT[0:rows, 0:1], scale=scale),
                          reads=[PSB[pss], B_const], writes=[B_rs[s]])
                    sy.op("act", lambda: A_.activation(out=rs[s][0:rows, 0:n], in_=rs[s][0:rows, 0:n], func=AF.Exp,
                                                       scale=-0.5),
                          reads=[B_rs[s]], writes=[B_rs[s]])
                    return rs[s], B_rs[s]

                def proj(pi, c0, m, hs, n):
                    for k in range(8):
                        sy.op("pe", lambda k=k: PE.matmul(PS[pi][0:m, 0:n], win[:, k, c0:c0 + m], hTb[hs][:, k, 0:n],
                                                          start=(k == 0), stop=(k == 7)),
                              reads=[B_w, B_hTb[hs]], writes=[PSB[pi]], mark=(k == 7))

                pending2 = []
                picnt = [0]
                ubcnt = [0]
                sqcnt = [0]

                def flush2():
                    while pending2:
                        pending2.pop(0)()

                for bi, (c0, n) in enumerate(BLOCKS):
                    hs = bi % 2
                    tiles = [c0 // 128 + i for i in range(n // 128)]
                    sy.dma("sp", hTb[hs][:, :, 0:n], hTd[:, :, c0:c0 + n], reads=[db("hT", t) for t in tiles],
                           writes=[B_hTb[hs]])
                    isctx = (bi == 0)
                    if gl:
                        sy.dma("sp", cs[hs][:, 0, 0:n], I["cosH"][:, c0:c0 + n], writes=[B_cs[hs]])
                        sy.dma("sp", cs[hs][:, 1, 0:n], I["sinH"][:, c0:c0 + n], writes=[B_cs[hs]])
                        for ch in range(5):
                            if ch < 3 and isctx and not with_ctx:
                                continue
                            picnt[0] += 1
                            pi = picnt[0] % 2
                            proj(pi, ch * 128, 128, hs, n)
                            if ch < 3:
                                dst = qT[:, ch, c0:c0 + n]
                                dbuf = bq((ch, bi))
                            else:
                                dst = kT[:, ch - 3, c0:c0 + n]
                                dbuf = bk((ch - 3, bi))
                            if mx == "A":
                                gcol = lc + (0 if ch < 3 else 1)
                                sqcnt[0] += 1
                                s3 = sqcnt[0] % 3
                                sy.op("act", lambda pi=pi, s3=s3: A_.activation(
                                    out=sq[s3][:, 0:n], in_=PS[pi][:, 0:n], func=AF.Square),
                                    reads=[PSB[pi]], writes=[B_sq[s3]])
                                s_ = rope_s1(pi, n, 128, gcol)

                                def rst_fn(s3=s3, n=n):
                                    pss = 6 + nxt(2)
                                    sy.op("pe", lambda: PE.matmul(PS[pss][:, 0:n], BONES, sq[s3][:, 0:n],
                                                                  start=True, stop=True),
                                          reads=[B_sq[s3], B_const], writes=[PSB[pss]])
                                    return rstd_from(pss, n, 1.0 / HD)

                                st2 = (lambda s_=s_, n=n, dst=dst, dbuf=dbuf, hs=hs, rst_fn=rst_fn, gcol=gcol:
                                       rope_s2(s_, n, dst, dbuf, cs[hs], B_cs[hs], 128, PM128, rstd=rst_fn,
                                               gain_col=gcol))
                            else:
                                s_ = rope_s1(pi, n, 128, None)
                                st2 = (lambda s_=s_, n=n, dst=dst, dbuf=dbuf, hs=hs:
                                       rope_s2(s_, n, dst, dbuf, cs[hs], B_cs[hs], 128, PM128))
                            flush2()
                            pending2.append(st2)
                        for ti, t in enumerate(tiles):
                            pv = 2 + nxt(2)
                            for k in range(8):
                                sy.op("pe", lambda k=k, ti=ti, pv=pv: PE.matmul(
                                    PS[pv][:, 0:128], hTb[hs][:, k, ti * 128:(ti + 1) * 128], win[:, k, 640:768],
                                    start=(k == 0), stop=(k == 7)), reads=[B_w, B_hTb[hs]], writes=[PSB[pv]],
                                    mark=(k == 7))
                            sy.op("dve", lambda t=t, pv=pv: V.tensor_copy(
                                out=Va[:, t, :, 64:128], in_=PS[pv][:, 0:128].rearrange("p (h d) -> p h d", h=2)),
                                reads=[PSB[pv], B_vones], writes=[B_v[t]])
                    else:
                        sy.dma("sp", cs[hs][:, 0, 0:n], I["cosR"][:, c0:c0 + n], writes=[B_cs[hs]])
                        sy.dma("sp", cs[hs][:, 1, 0:n], I["sinR"][:, c0:c0 + n], writes=[B_cs[hs]])
                        sy.dma("sp", csK[hs][:, 0, 0:n], I["cosK"][:, c0:c0 + n], writes=[B_cs[hs]])
                        sy.dma("sp", csK[hs][:, 1, 0:n], I["sinK"][:, c0:c0 + n], writes=[B_cs[hs]])
                        need_q = (not isctx) or with_ctx
                        pss_q = 6
                        pss_kv = 7
                        for ch in range(3):
                            if ch < 2 and not need_q:
                                continue
                            pi = nxt(2)
                            proj(pi, ch * 128, 128, hs, n)
                            s3 = nxt(3)
                            sy.op("act", lambda pi=pi, s3=s3: A_.activation(
                                out=sq[s3][:, 0:n], in_=PS[pi][:, 0:n], func=AF.Square),
                                reads=[PSB[pi]], writes=[B_sq[s3]])
                            gcol = lc + 2 + ch
                            sy.op("act", lambda pi=pi, ch=ch, gcol=gcol: A_.activation(
                                out=cqf[ch][:, 0:n], in_=PS[pi][:, 0:n], func=AF.Copy,
                                scale=pvec[:, gcol:gcol + 1]), reads=[PSB[pi], B_const], writes=[B_cqf[ch]])
                            if ch < 2:
                                sy.op("pe", lambda s3=s3, ch=ch: PE.matmul(PS[pss_q][:, 0:n], AONES, sq[s3][:, 0:n],
                                                                           start=(ch == 0), stop=(ch == 1)),
                                      reads=[B_sq[s3], B_const], writes=[PSB[pss_q]], mark=(ch == 1))
                            else:
                                sy.op("pe", lambda s3=s3: PE.matmul(PS[pss_kv][:, 0:n], AONES, sq[s3][:, 0:n],
                                                                    start=True, stop=True),
                                      reads=[B_sq[s3], B_const], writes=[PSB[pss_kv]])
                        if need_q:
                            rt, B_rt = rstd_from(pss_q, n, 1.0 / 256)
                            for ch in range(2):
                                sy.op("dve", lambda ch=ch, rt=rt: V.tensor_tensor(
                                    out=cqn[ch][:, 0:n], in0=cqf[ch][:, 0:n], in1=rt[:, 0:n], op=ALU.mult),
                                    reads=[B_cqf[ch], B_rt], writes=[B_cqn[ch]])
                        rt, B_rt = rstd_from(pss_kv, n, 1.0 / 128)
                        sy.op("dve", lambda rt=rt: V.tensor_tensor(
                            out=cqn[2][:, 0:n], in0=cqf[2][:, 0:n], in1=rt[:, 0:n], op=ALU.mult),
                            reads=[B_cqf[2], B_rt], writes=[B_cqn[2]])
                        pi = nxt(2)
                        proj(pi, 384, 32, hs, n)
                        rope_store(pi, n, krf[:, 0:n], B_krf, csK[hs], B_cs[hs], 32, PM32)
                        if need_q:
                            for h in range(4):
                                pi = nxt(2)
                                for c2 in range(2):
                                    sy.op("pe", lambda c2=c2, h=h, pi=pi: PE.matmul(
                                        PS[pi][:, 0:n], wuq[:, c2, h * 128:(h + 1) * 128], cqn[c2][:, 0:n],
                                        start=(c2 == 0), stop=(c2 == 1)), reads=[B_w, B_cqn[c2]], writes=[PSB[pi]],
                                        mark=(c2 == 1))
                                rope_store(pi, n, qT[:, h, c0:c0 + n], bq((h, bi)), cs[hs], B_cs[hs], 128, PM96)
                        for h in range(4):
                            pi = nxt(2)
                            sy.op("pe", lambda h=h, pi=pi: PE.matmul(
                                PS[pi][:, 0:n], wuk[:, h * 128:(h + 1) * 128], cqn[2][:, 0:n], start=True,
                                stop=False), reads=[B_w, B_cqn[2]], writes=[PSB[pi]], mark=False)
                            sy.op("pe", lambda pi=pi: PE.matmul(
                                PS[pi][:, 0:n], SEL32, krf[:, 0:n], start=False, stop=True),
                                reads=[B_const, B_krf], writes=[PSB[pi]])
                            sy.op("act", lambda h=h, pi=pi: A_.copy(out=kT[:, h, c0:c0 + n], in_=PS[pi][:, 0:n]),
                                  reads=[PSB[pi]], writes=[bk((h, bi))])
                        for ti, t in enumerate(tiles):
                            pv = 2 + nxt(2)
                            sy.op("pe", lambda ti=ti, pv=pv: PE.matmul(
                                PS[pv][:, 0:256], cqn[2][:, ti * 128:(ti + 1) * 128], wuv[:], start=True, stop=True),
                                reads=[B_w, B_cqn[2]], writes=[PSB[pv]])
                            for h in range(4):
                                o = 0 if h % 2 == 0 else 64
                                sy.op("dve", lambda t=t, pv=pv, h=h, o=o: V.tensor_copy(
                                    out=Va[:, t, h, o:o + 64], in_=PS[pv][:, h * 64:(h + 1) * 64]),
                                    reads=[PSB[pv], B_vones], writes=[B_v[t]])

                flush2()
                nheads = 6 if gl else 4
                scale = (HD ** -0.5) if gl else (96 ** -0.5)
                GS = 3
                pt = [sb(st, "pt%d" % i, [128, GS, 512], BF16) for i in range(2)]
                B_pt = [sy.buf("pt%d" % i) for i in range(2)]
                rec = [sb(st, "rec%d" % i, [128, 512], F32) for i in range(2)]
                B_rec = [sy.buf("rec%d" % i) for i in range(2)]
                mo = [sb(st, "mo%d" % i, [128, 512], BF16) for i in range(2)]
                B_mo = [sy.buf("mo%d" % i, dma=True) for i in range(2)]
                OPS = [6, 7]
                qblocks = list(range(1, 9)) + ([0] if with_ctx else [])
                def make_item(qb, h, it):
                    c0, n = BLOCKS[qb]
                    if gl:
                        kvh = h // 3
                        par = h % 2
                        base = par * 64
                        K = 64
                        q_ap = qT[base:base + 64, h // 2, c0:c0 + n]
                        q_buf = bq((h // 2, qb))
                        kidx = 0 if (kvh == par) else 1
                        mixchunk = (0 if mx == "A" else 5) + h // 2
                    else:
                        kvh = h
                        par = h % 2
                        base = 0
                        K = 128
                        q_ap = qT[:, h, c0:c0 + n]
                        q_buf = bq((h, qb))
                        kidx = h
                        mixchunk = 3 + h // 2
                    if qb == 0:
                        kl = [(0, None), (1, None)]
                    elif mx == "C":
                        l = qb - 1
                        kl = [(0, None), (1, None)]
                        for ci, cc in enumerate(range(-1, 5)):
                            j = 4 * l + cc
                            if 0 <= j < 32:
                                kl.append((2 + j, ci))
                    else:
                        kl = [(t, None) for t in range(NT)]
                    po = OPS[it % 2]
                    nk = len(kl)
                    sink = (mx == "C")

                    groups = [kl[i:i + GS] for i in range(0, nk, GS)]
                    ng = len(groups)

                    def qkg(j, sl_):
                        for ci, (t, _) in enumerate(groups[j]):
                            bank = GS * sl_ + ci
                            kb = 0 if t < 2 else 1 + (t - 2) // 4
                            k_ap = kT[base:base + K, kidx, t * 128:(t + 1) * 128]
                            sy.op("pe", lambda bank=bank, k_ap=k_ap: PE.matmul(
                                PS[bank][:, 0:n], k_ap, q_ap, start=True, stop=True),
                                reads=[bk((kidx, kb)), q_buf], writes=[PSB[bank]],
                                mark=(ci == len(groups[j]) - 1))

                    def pvg(j, sl_):
                        g = len(groups[j])
                        src = PSALL[:, GS * sl_ * 512:(GS * sl_ + g) * 512].rearrange("p (c n) -> p c n", c=g)
                        sy.op("act", lambda: A_.activation(out=pt[sl_][:, 0:g, 0:n], in_=src[:, :, 0:n],
                                                           func=AF.Exp, scale=scale),
                              reads=[PSB[GS * sl_ + ci] for ci in range(g)], writes=[B_pt[sl_]])
                        for ci, (t, mid) in enumerate(groups[j]):
                            if mid is not None:
                                sy.op("pool", lambda ci=ci, mid=mid: G_.tensor_tensor(
                                    out=pt[sl_][:, ci, 0:n], in0=pt[sl_][:, ci, 0:n], in1=wmask[:, mid, 0:n],
                                    op=ALU.mult), reads=[B_pt[sl_], B_wmask], writes=[B_pt[sl_]])
                        for ci, (t, mid) in enumerate(groups[j]):
                            if gl:
                                v_ap = Va[:, t, kvh, 64:192] if par == 0 else Va[:, t, kvh, 0:128]
                            else:
                                v_ap = Va[:, t, h, :]
                            first_ = (j == 0 and ci == 0)
                            last_ = (j == ng - 1 and ci == g - 1)
                            sy.op("pe", lambda ci=ci, v_ap=v_ap, first_=first_, last_=last_: PE.matmul(
                                PS[po][:, 0:n], v_ap, pt[sl_][:, ci, 0:n], start=first_, stop=last_),
                                reads=[B_v[t], B_vones, B_pt[sl_]], writes=[PSB[po]], mark=(ci == g - 1))

                    def epi():
                        orow = slice(0, 64) if par == 0 else slice(64, 128)
                        srow = slice(64, 128) if par == 0 else slice(0, 64)
                        s2 = it % 2
                        if sink:
                            sy.op("dve", lambda: V.tensor_copy(out=rec[s2][orow, 0:n], in_=PS[po][srow, 0:n]),
                                  reads=[PSB[po]], writes=[B_rec[s2]])
                            sy.op("dve", lambda: V.tensor_scalar(out=rec[s2][orow, 0:n], in0=rec[s2][orow, 0:n],
                                                                 scalar1=sk[orow, h:h + 1], scalar2=None, op0=ALU.add),
                                  reads=[B_rec[s2], B_sk], writes=[B_rec[s2]])
                            sy.op("dve", lambda: V.reciprocal(out=rec[s2][orow, 0:n], in_=rec[s2][orow, 0:n]),
                                  reads=[B_rec[s2]], writes=[B_rec[s2]])
                        else:
                            sy.op("dve", lambda: V.reciprocal(out=rec[s2][orow, 0:n], in_=PS[po][srow, 0:n]),
                                  reads=[PSB[po]], writes=[B_rec[s2]])
                        sy.op("dve", lambda: V.tensor_tensor(out=mo[s2][orow, 0:n], in0=PS[po][orow, 0:n],
                                                             in1=rec[s2][orow, 0:n], op=ALU.mult),
                              reads=[PSB[po], B_rec[s2]], writes=[B_mo[s2]])
                        sy.dma("sp", S["mixT"][mixchunk, orow, c0:c0 + n], mo[s2][orow, 0:n], reads=[B_mo[s2]],
                               writes=[db("mixT", (mixchunk, par, qb))])

                    return (ng, qkg, pvg, epi)

                items = [make_item(qb, h, idx) for idx, (qb, h) in
                         enumerate([(qb, h) for qb in qblocks for h in range(nheads)])]
                seq = [(im, j) for im in items for j in range(im[0])]
                if seq:
                    seq[0][0][1](seq[0][1], 0)
                for x, (im, j) in enumerate(seq):
                    if x + 1 < len(seq):
                        nim, nj = seq[x + 1]
                        nim[1](nj, (x + 1) % 2)
                    im[2](j, x % 2)
                    if j == im[0] - 1:
                        im[3]()
                sy.barrier()

        def phase_wout(li, Xsrc, Xdst, tile_from):
            with contextlib.ExitStack() as st:
                md = load_mod(st, [("GT", 2048)])
                GTt, B_GT = md["GT"]
                wo = sb(st, "wo", [128, 8, D], BF16)
                B_wo = sy.buf("wo", sw=True)
                sy.dma("pool", wo[:], I["w_out"][li].rearrange("(k p) c -> p k c", p=128), writes=[B_wo])
                mT = [sb(st, "mT%d" % i, [128, 8, 512], BF16) for i in range(2)]
                B_mT = [sy.buf("mT%d" % i, dma=True) for i in range(2)]
                xt = [sb(st, "xt%d" % i, [128, D], F32) for i in range(3)]
                B_xt = [sy.buf("xt%d" % i, dma=True) for i in range(3)]
                tm = [sb(st, "tm%d" % i, [128, D], F32) for i in range(2)]
                B_tm = [sy.buf("tm%d" % i) for i in range(2)]
                mTd = S["mixT"].rearrange("k p t -> p k t")
                blks = []
                jobs = []
                for bi, (c0, n) in enumerate(BLOCKS):
                    tiles = [c0 // 128 + i for i in range(n // 128)]
                    if tiles[0] < tile_from:
                        continue
                    blks.append((bi, c0, n))
                    for ti, t in enumerate(tiles):
                        jobs.append((len(blks) - 1, ti, t))

                def load_blk(bx):
                    bi, c0, n = blks[bx]
                    hs = bx % 2
                    rd = [db("mixT", (ch, par, bi)) for ch in range(8) for par in range(2)]
                    sy.dma("sp", mT[hs][:, :, 0:n], mTd[:, :, c0:c0 + n], reads=rd, writes=[B_mT[hs]])

                def load_x(job):
                    bx, ti, t = job
                    s3 = t % 3
                    sy.dma("sp", xt[s3][:], Xsrc[t * 128:(t + 1) * 128, :], reads=[db(Xsrc.name, t)],
                           writes=[B_xt[s3]])

                def compute(job):
                    bx, ti, t = job
                    hs = bx % 2
                    ty = 1 if t < 2 else 0
                    s3 = t % 3
                    s2 = t % 2
                    for dh in range(2):
                        pi = (2 * t + dh) % 4
                        for k in range(8):
                            sy.op("pe", lambda k=k, pi=pi, dh=dh: PE.matmul(
                                PS[pi][:], mT[hs][:, k, ti * 128:(ti + 1) * 128], wo[:, k, dh * 512:(dh + 1) * 512],
                                start=(k == 0), stop=(k == 7)), reads=[B_mT[hs], B_wo], writes=[PSB[pi]],
                                mark=(k == 7))
                        sy.op("dve", lambda pi=pi, dh=dh, ty=ty: V.tensor_tensor(
                            out=tm[s2][:, dh * 512:(dh + 1) * 512], in0=PS[pi][:],
                            in1=GTt[:, ty, dh * 512:(dh + 1) * 512], op=ALU.mult),
                            reads=[PSB[pi], B_GT], writes=[B_tm[s2]])
                    sy.op("pool", lambda s3=s3, s2=s2: G_.tensor_tensor(out=xt[s3][:], in0=xt[s3][:], in1=tm[s2][:],
                                                                        op=ALU.add),
                          reads=[B_xt[s3], B_tm[s2]], writes=[B_xt[s3]])
                    sy.dma("sp", Xdst[t * 128:(t + 1) * 128, :], xt[s3][:], reads=[B_xt[s3]],
                           writes=[db(Xdst.name, t)])

                nj = len(jobs)
                load_blk(0)
                for q in range(min(3, nj)):
                    load_x(jobs[q])
                for q in range(nj):
                    bx, ti, t = jobs[q]
                    if ti == 0 and bx + 1 < len(blks):
                        load_blk(bx + 1)
                    compute(jobs[q])
                    if q + 3 < nj:
                        load_x(jobs[q + 3])
                sy.barrier()

        def phase_ffn(li, Xsrc, Xdst, tile_from, moe, final):
            ntok_tiles = NT - tile_from
            half = ntok_tiles // 2
            supers = [(tile_from, half), (tile_from + half, ntok_tiles - half)]
            dff = DFF_E if moe else DFF_D
            nfc = dff // 128
            groups = [(g0, min(4, nfc - g0)) for g0 in range(0, nfc, 4)]
            nexp = NE if moe else 1
            with contextlib.ExitStack() as st:
                md = load_mod(st, [("GT", 5120)])
                GTt, B_GT = md["GT"]
                maxst = max(s[1] for s in supers)
                h2 = sb(st, "h2", [128, 8, maxst * 128], BF16)
                yacc = sb(st, "yacc", [128, maxst, D], F32)
                B_h2 = sy.buf("h2", dma=True)
                B_y = [sy.buf("y%d" % i) for i in range(maxst)]
                W1 = [sb(st, "W1_%d" % i, [128, 8, 512], BF16) for i in range(2)]
                W3 = [sb(st, "W3_%d" % i, [128, 8, 512], BF16) for i in range(2)]
                W2 = [sb(st, "W2_%d" % i, [128, 4, D], BF16) for i in range(2)]
                B_W = [sy.buf("W%d" % i, sw=True) for i in range(2)]
                sa = [sb(st, "sa%d" % i, [128, 512], BF16) for i in range(2)]
                B_sa = [sy.buf("sa%d" % i) for i in range(2)]
                gg = [sb(st, "gg%d" % i, [128, 4, 512], BF16) for i in range(2)]
                B_gg = [[sy.buf("gg%d_%d" % (i, f)) for f in range(4)] for i in range(2)]
                xt = [sb(st, "xt%d" % i, [128, D], F32) for i in range(2)]
                B_xt = [sy.buf("xt%d" % i, dma=True) for i in range(2)]
                if final:
                    fn = sb(st, "fn", [128, D], F32)
                    B_fn = sy.buf("fn", dma=True)
                    sy.dma("sp", fn[:], I["final_norm"].partition_broadcast(128), writes=[B_fn])
                    junk = sb(st, "junk", [128, D], BF16)
                    B_junk = sy.buf("junk")
                    stat = [sb(st, "stat%d" % i, [128, 4], F32) for i in range(2)]
                    B_stat = [sy.buf("stat%d" % i) for i in range(2)]
                hTd = S["hT"].rearrange("k p t -> p k t")
                gi = 0
                def load_h2(si, q):
                    t0_, ntl_ = supers[si]
                    sy.dma(q, h2[:, :, 0:ntl_ * 128], hTd[:, :, t0_ * 128:(t0_ + ntl_) * 128],
                           reads=[db("hT", t) for t in range(t0_, t0_ + ntl_)], writes=[B_h2])

                load_h2(0, "sp")
                for si, (t0, ntl) in enumerate(supers):
                    ntok = ntl * 128
                    tbs = [(o, min(512, ntok - o)) for o in range(0, ntok, 512)]
                    first = True
                    for e in range(nexp):
                        if moe:
                            w1s = I["w1_moe"][0, e]
                            w3s = I["w3_moe"][0, e]
                            w2s = I["w2_moe"][0, e]
                        else:
                            w1s = I["w1_dense"][0]
                            w3s = I["w3_dense"][0]
                            w2s = I["w2_dense"][0]
                        w1r = w1s.rearrange("(k p) c -> p k c", p=128)
                        w3r = w3s.rearrange("(k p) c -> p k c", p=128)
                        w2r = w2s.rearrange("(f p) d -> p f d", p=128)
                        for (g0, gn) in groups:
                            ws = gi % 2
                            gi += 1
                            sy.dma("pool", W1[ws][:, :, 0:gn * 128], w1r[:, :, g0 * 128:(g0 + gn) * 128],
                                   writes=[B_W[ws]])
                            sy.dma("pool", W3[ws][:, :, 0:gn * 128], w3r[:, :, g0 * 128:(g0 + gn) * 128],
                                   writes=[B_W[ws]])
                            sy.dma("pool", W2[ws][:, 0:gn, :], w2r[:, g0:g0 + gn, :], writes=[B_W[ws]])
                            for tbi, (o, n) in enumerate(tbs):
                                gs = tbi % 2
                                for f in range(gn):
                                    pa = (2 * f) % 4
                                    pb = (2 * f + 1) % 4
                                    for (pp, Wt) in ((pa, W1), (pb, W3)):
                                        for k in range(8):
                                            sy.op("pe", lambda k=k, pp=pp, Wt=Wt, f=f: PE.matmul(
                                                PS[pp][:, 0:n], Wt[ws][:, k, f * 128:(f + 1) * 128],
                                                h2[:, k, o:o + n], start=(k == 0), stop=(k == 7)),
                                                reads=[B_W[ws], B_h2], writes=[PSB[pp]], mark=(k == 7))
                                    s2 = f % 2
                                    sy.op("act", lambda pa=pa, s2=s2: A_.activation(
                                        out=sa[s2][:, 0:n], in_=PS[pa][:, 0:n], func=AF.Silu),
                                        reads=[PSB[pa]], writes=[B_sa[s2]])
                                    sy.op("dve", lambda pb=pb, s2=s2, f=f, gs=gs: V.tensor_tensor(
                                        out=gg[gs][:, f, 0:n], in0=PS[pb][:, 0:n], in1=sa[s2][:, 0:n], op=ALU.mult),
                                        reads=[PSB[pb], B_sa[s2]], writes=[B_gg[gs][f]])
                                for ts in range(n // 128):
                                    tl = (o // 128) + ts
                                    for dh in range(2):
                                        py = 4 + (2 * ts + dh) % 4
                                        for f in range(gn):
                                            sy.op("pe", lambda f=f, py=py, ts=ts, dh=dh, gs=gs: PE.matmul(
                                                PS[py][:], gg[gs][:, f, ts * 128:(ts + 1) * 128],
                                                W2[ws][:, f, dh * 512:(dh + 1) * 512], start=(f == 0),
                                                stop=(f == gn - 1)), reads=[B_gg[gs][f], B_W[ws]],
                                                writes=[PSB[py]], mark=(f == gn - 1))
                                        ya = yacc[:, tl, dh * 512:(dh + 1) * 512]
                                        if moe:
                                            cj = t0 - 2 + tl
                                            if first:
                                                sy.op("dve", lambda py=py, ya=ya, cj=cj, e=e: V.tensor_scalar(
                                                    out=ya, in0=PS[py][:], scalar1=comb[:, cj, e:e + 1], scalar2=None,
                                                    op0=ALU.mult), reads=[PSB[py], B_comb[cj]], writes=[B_y[tl]])
                                            else:
                                                sy.op("dve", lambda py=py, ya=ya, cj=cj, e=e: V.scalar_tensor_tensor(
                                                    out=ya, in0=PS[py][:], scalar=comb[:, cj, e:e + 1], in1=ya,
                                                    op0=ALU.mult, op1=ALU.add), reads=[PSB[py], B_comb[cj], B_y[tl]],
                                                    writes=[B_y[tl]])
                                        else:
                                            if first:
                                                sy.op("dve", lambda py=py, ya=ya: V.tensor_copy(out=ya, in_=PS[py][:]),
                                                      reads=[PSB[py]], writes=[B_y[tl]])
                                            else:
                                                sy.op("dve", lambda py=py, ya=ya: V.tensor_tensor(
                                                    out=ya, in0=PS[py][:], in1=ya, op=ALU.add),
                                                    reads=[PSB[py], B_y[tl]], writes=[B_y[tl]])
                            first = False
                    if si + 1 < len(supers):
                        load_h2(si + 1, "act")
                    def ep_load(tl):
                        t = t0 + tl
                        sy.dma("sp", xt[tl % 2][:], Xsrc[t * 128:(t + 1) * 128, :], reads=[db(Xsrc.name, t)],
                               writes=[B_xt[tl % 2]])

                    ep_load(0)
                    for tl in range(ntl):
                        t = t0 + tl
                        ty = 1 if t < 2 else 0
                        s2 = tl % 2
                        if tl + 1 < ntl:
                            ep_load(tl + 1)
                        sy.op("dve", lambda tl=tl, ty=ty: V.tensor_tensor(out=yacc[:, tl, :], in0=yacc[:, tl, :],
                                                                          in1=GTt[:, ty, :], op=ALU.mult),
                              reads=[B_y[tl], B_GT], writes=[B_y[tl]])
                        sy.op("pool", lambda tl=tl, s2=s2: G_.tensor_tensor(out=xt[s2][:], in0=xt[s2][:],
                                                                            in1=yacc[:, tl, :], op=ALU.add),
                              reads=[B_xt[s2], B_y[tl]], writes=[B_xt[s2]])
                        if not final:
                            sy.dma("sp", Xdst[t * 128:(t + 1) * 128, :], xt[s2][:], reads=[B_xt[s2]],
                                   writes=[db(Xdst.name, t)])
                        else:
                            sy.op("act", lambda s2=s2: A_.activation(out=junk[:], in_=xt[s2][:], func=AF.Square,
                                                                     accum_out=stat[s2][:, 0:1]),
                                  reads=[B_xt[s2]], writes=[B_junk, B_stat[s2]])
                            sy.op("act", lambda s2=s2: A_.activation(out=stat[s2][:, 1:2], in_=stat[s2][:, 0:1],
                                                                     func=AF.Sqrt, bias=epsT[:, 0:1], scale=1.0 / D),
                                  reads=[B_stat[s2], B_const], writes=[B_stat[s2]])
                            sy.op("dve", lambda s2=s2: V.reciprocal(out=stat[s2][:, 2:3], in_=stat[s2][:, 1:2]),
                                  reads=[B_stat[s2]], writes=[B_stat[s2]])
                            sy.op("dve", lambda s2=s2: V.scalar_tensor_tensor(
                                out=xt[s2][:], in0=xt[s2][:], scalar=stat[s2][:, 2:3], in1=fn[:], op0=ALU.mult,
                                op1=ALU.mult), reads=[B_xt[s2], B_stat[s2], B_fn], writes=[B_xt[s2]])
                            lt = t - 2
                            sy.dma("sp", out_d[lt * 128:(lt + 1) * 128, :], xt[s2][:], reads=[B_xt[s2]],
                                   writes=[db("out", lt)])
                sy.barrier()

        phases = []
        Xin = I["xin"]
        phases.append(("mod0", lambda: phase_mod(0)))
        phases.append(("n1_0", lambda: phase_norm(0, 0, Xin, 0)))
        for mx in "ABC":
            phases.append(("mix%s0" % mx, lambda mx=mx: phase_mixer(0, mx, True)))
        phases.append(("wo0", lambda: phase_wout(0, Xin, S["X1"], 0)))
        phases.append(("n2_0", lambda: phase_norm(0, 1, S["X1"], 0)))
        phases.append(("ff0", lambda: phase_ffn(0, S["X1"], S["X2"], 0, False, False)))
        phases.append(("mod1", lambda: phase_mod(1)))
        phases.append(("n1_1", lambda: phase_norm(1, 0, S["X2"], 0)))
        for mx in "ABC":
            phases.append(("mix%s1" % mx, lambda mx=mx: phase_mixer(1, mx, False)))
        phases.append(("wo1", lambda: phase_wout(1, S["X2"], S["X3"], 2)))
        phases.append(("n2_1", lambda: phase_norm(1, 1, S["X3"], 2, with_router=True)))
        phases.append(("ff1", lambda: phase_ffn(1, S["X3"], S["X4"], 2, True, True)))
        for name, fn_ in phases:
            if name in skip:
                continue
            fn_()
            PHASE_MARKS.append((name, sy.cnt["pe"]))
            if stop_after == name:
                break
        sy.barrier()
    return nc


def prep_inputs(inputs, b):
    f = lambda a: np.ascontiguousarray(np.asarray(a, dtype=np.float32))
    m = {}
    m["xin"] = f(np.concatenate([inputs["ctx"][b], inputs["x"][b]], axis=0))
    cv = np.zeros((128, 16), np.float32)
    cv[:, 0:8] = np.asarray(inputs["c"][b]).reshape(8, 128).T
    cv[:, 8:16] = np.asarray(inputs["c_ctx"]).reshape(8, 128).T
    m["cvec"] = cv
    return m


def prep_shared(inputs):
    f = lambda a: np.ascontiguousarray(np.asarray(a, dtype=np.float32))
    m = {}
    for k in ("w_mod", "b_mod", "norm_mix", "norm_ffn", "c_sinks", "w_out", "w1_dense", "w3_dense",
              "w2_dense", "w_router", "w1_moe", "w3_moe", "w2_moe", "final_norm"):
        m[k] = f(inputs[k])
    w_in = np.asarray(inputs["w_in"], dtype=np.float32)
    pa = w_in[:, :, 0:640]
    pb = w_in[:, :, 640:1056]
    pc = w_in[:, :, 1056:1696]

    def gqa_cols(p):
        q = p[:, :, 0:384]
        k0 = p[:, :, 384:448]
        k1 = p[:, :, 448:512]
        v = p[:, :, 512:640]
        return np.concatenate([q, k0, k1, k1, k0, v], axis=2)

    m["wA"] = f(gqa_cols(pa))
    m["wB"] = f(pb)
    m["wC"] = f(gqa_cols(pc))
    pv = np.zeros((128, 16), np.float32)
    aq = np.asarray(inputs["a_q_norm"], np.float32)
    ak = np.asarray(inputs["a_k_norm"], np.float32)
    bqn = np.asarray(inputs["b_q_norm"], np.float32)
    bkv = np.asarray(inputs["b_kv_norm"], np.float32)
    for li in range(DEPTH):
        pv[:, li * 5 + 0] = np.concatenate([aq[li], aq[li]])
        pv[:, li * 5 + 1] = np.concatenate([ak[li], ak[li]])
        pv[:, li * 5 + 2] = bqn[li, 0:128]
        pv[:, li * 5 + 3] = bqn[li, 128:256]
        pv[:, li * 5 + 4] = bkv[li]
    m["pvec"] = pv
    wukv = np.asarray(inputs["w_ukv"], np.float32).reshape(DEPTH, 128, 4, 128)
    wk = np.zeros((DEPTH, 128, 4, 128), np.float32)
    wk[:, :, :, 0:64] = wukv[:, :, :, 0:64]
    m["wukv_k"] = f(wk.reshape(DEPTH, 128, 512))
    wq = np.zeros((DEPTH, 256, 4, 128), np.float32)
    wq[:, :, :, 0:96] = np.asarray(inputs["w_uq"], np.float32).reshape(DEPTH, 256, 4, 96)
    m["w_uq"] = f(wq.reshape(DEPTH, 256, 512))
    m["wukv_v"] = f(wukv[:, :, :, 64:128].reshape(DEPTH, 128, 256))
    m.update(make_consts())
    return m


_CACHE = {}


def kernel(**inputs):
    inputs = {k: np.asarray(v) for k, v in inputs.items()}
    if "nc" not in _CACHE:
        _CACHE["nc"] = build()
    nc = _CACHE["nc"]
    shared = prep_shared(inputs)
    in_maps = []
    for b in range(8):
        m = dict(shared)
        m.update(prep_inputs(inputs, b))
        in_maps.append(m)
    res = run_bass_kernel_spmd(nc, in_maps, core_ids=list(range(8)))
    out = np.stack([np.asarray(r["out"], dtype=np.float32) for r in res.results], axis=0)
    return out
```
